# Optimizing a Trainium2 kernel written in Bass

```python
import jax, jax.numpy as jnp
from jax import lax
import numpy as np

D_MODEL = 1024
BATCH = 16
SEQ = 2048
DEPTH = 1

D_MIX = D_MODEL
D_MLSTM = D_MIX // 2
D_RWKV = D_MIX - D_MLSTM
MLSTM_HEADS = 4
MLSTM_HEAD_DIM = D_MLSTM // MLSTM_HEADS
MLSTM_CHUNK = 128
CONV_WIDTH = 4
RWKV_HEAD_DIM = 64
RWKV_HEADS = D_RWKV // RWKV_HEAD_DIM
DECAY_LORA = 64
ICLR_LORA = 64
GATE_LORA = 128
D_IN_MLSTM = 4 * D_MLSTM + 2 * MLSTM_HEADS
D_IN_RWKV = 3 * D_RWKV + DECAY_LORA + ICLR_LORA + GATE_LORA
D_IN = D_IN_MLSTM + D_IN_RWKV
D_FF = -(-8 * D_MODEL // (3 * 256)) * 256
MEM_TOKENS = 256
XATTN_HEADS = 4
XATTN_HEAD_DIM = D_MODEL // XATTN_HEADS
EPS = 1e-6
RWKV_LN_EPS = 64e-5

kernel_name = 'hybrid_mlstm_rwkv7_memxattn_block'


def rms_norm(x, g):
    xf = x.astype(jnp.float32)
    y = xf * lax.rsqrt(jnp.mean(xf * xf, -1, keepdims=True) + EPS)
    return (y * g.astype(jnp.float32)).astype(x.dtype)


def split_cols(z, sizes):
    outs, start = [], 0
    for s in sizes:
        outs.append(z[..., start:start + s])
        start += s
    return outs


def token_shift(z):
    return jnp.pad(z, ((0, 0), (1, 0), (0, 0)))[:, :-1]


def causal_conv(z, w):
    T = z.shape[1]
    zp = jnp.pad(z, ((0, 0), (CONV_WIDTH - 1, 0), (0, 0)))
    out = zp[:, 0:T] * w[0]
    for j in range(1, CONV_WIDTH):
        out = out + zp[:, j:j + T] * w[j]
    return out


def mlstm_chunkwise(q, k, v, i_pre, f_pre):
    B, T, H, DH = q.shape
    L = MLSTM_CHUNK
    NC = T // L
    f32 = jnp.float32
    to_chunks = lambda t: t.astype(f32).reshape(B, NC, L, H, DH).transpose(0, 3, 1, 2, 4)
    q = to_chunks(q)
    k = to_chunks(k) * (DH ** -0.5)
    v = to_chunks(v)
    ig = i_pre.astype(f32).reshape(B, NC, L, H).transpose(0, 3, 1, 2)
    lf = jax.nn.log_sigmoid(f_pre.astype(f32)).reshape(B, NC, L, H).transpose(0, 3, 1, 2)
    b = jnp.cumsum(lf, axis=-1)
    g = b[..., -1]

    a = g[..., None] - b + ig
    m_loc = jnp.max(a, -1)
    wgt = jnp.exp(a - m_loc[..., None])
    dC = jnp.einsum('bhcld,bhcle->bhcde', v * wgt[..., None], k)
    dn = jnp.einsum('bhcl,bhcle->bhce', wgt, k)

    def step(carry, inp):
        C, n, m = carry
        g_c, mloc_c, dC_c, dn_c = inp
        m_new = jnp.maximum(g_c + m, mloc_c)
        s_old = jnp.exp(g_c + m - m_new)
        s_new = jnp.exp(mloc_c - m_new)
        C_new = s_old[..., None, None] * C + s_new[..., None, None] * dC_c
        n_new = s_old[..., None] * n + s_new[..., None] * dn_c
        return (C_new, n_new, m_new), (C, n, m)

    init = (jnp.zeros((B, H, DH, DH), f32), jnp.zeros((B, H, DH), f32), jnp.zeros((B, H), f32))
    xs = (jnp.moveaxis(g, 2, 0), jnp.moveaxis(m_loc, 2, 0), jnp.moveaxis(dC, 2, 0), jnp.moveaxis(dn, 2, 0))
    _, (C_prev, n_prev, m_prev) = lax.scan(step, init, xs)
    C_prev = jnp.moveaxis(C_prev, 0, 2)
    n_prev = jnp.moveaxis(n_prev, 0, 2)
    m_prev = jnp.moveaxis(m_prev, 0, 2)

    causal = jnp.tril(jnp.ones((L, L), dtype=bool))
    Dm = b[..., :, None] - b[..., None, :] + ig[..., None, :]
    Dm = jnp.where(causal, Dm, -jnp.inf)
    m_intra = jnp.max(Dm, -1)
    m_inter = b + m_prev[..., None]
    m_t = jnp.maximum(m_inter, m_intra)
    S = jnp.einsum('bhcld,bhcsd->bhcls', q, k) * jnp.exp(Dm - m_t[..., None])
    s_inter = jnp.exp(m_inter - m_t)
    num = jnp.einsum('bhcls,bhcsd->bhcld', S, v) + s_inter[..., None] * jnp.einsum('bhcde,bhcle->bhcld', C_prev, q)
    den = jnp.sum(S, -1) + s_inter * jnp.einsum('bhce,bhcle->bhcl', n_prev, q)
    h = num / jnp.maximum(jnp.abs(den), jnp.exp(-m_t))[..., None]
    return h.transpose(0, 2, 3, 1, 4).reshape(B, T, H, DH)


def mlstm_group(z, conv_w, i_bias, f_bias, norm_w):
    B, T, _ = z.shape
    qk, v, o, ig, fg = split_cols(z, (2 * D_MLSTM, D_MLSTM, D_MLSTM, MLSTM_HEADS, MLSTM_HEADS))
    qk = jax.nn.silu(causal_conv(qk, conv_w))
    q, k = qk[..., :D_MLSTM], qk[..., D_MLSTM:]
    shp = (B, T, MLSTM_HEADS, MLSTM_HEAD_DIM)
    h = mlstm_chunkwise(q.reshape(shp), k.reshape(shp), v.reshape(shp), ig + i_bias, fg + f_bias)
    h = h * lax.rsqrt(jnp.mean(h * h, -1, keepdims=True) + EPS)
    h = h * norm_w.astype(jnp.float32).reshape(MLSTM_HEADS, MLSTM_HEAD_DIM)
    return (h.reshape(B, T, D_MLSTM) * jax.nn.sigmoid(o.astype(jnp.float32))).astype(z.dtype)


def rwkv7_scan(r, w, k, v, a, b):
    B, T, H, N = r.shape

    def step(S, inp):
        r_t, w_t, k_t, v_t, a_t, b_t = inp
        sa = jnp.einsum('bhij,bhj->bhi', S, a_t)
        S = S * w_t[:, :, None, :] + sa[..., None] * b_t[:, :, None, :] + v_t[..., None] * k_t[:, :, None, :]
        return S, jnp.einsum('bhij,bhj->bhi', S, r_t)

    xs = tuple(jnp.moveaxis(t, 1, 0) for t in (r, w, k, v, a, b))
    _, y = lax.scan(step, jnp.zeros((B, H, N, N), jnp.float32), xs)
    return jnp.moveaxis(y, 0, 1)


def rwkv7_group(z, mu, w0, w_up, a0, a_up, g_up, k_k, k_a, r_k, ln_w, ln_b):
    B, T, _ = z.shape
    f32 = jnp.float32
    z = z + (token_shift(z) - z) * mu
    r, k, v, xw, xa, xg = split_cols(z, (D_RWKV, D_RWKV, D_RWKV, DECAY_LORA, ICLR_LORA, GATE_LORA))
    w = -jax.nn.softplus(-(w0 + jnp.tanh(xw) @ w_up).astype(f32)) - 0.5
    decay = jnp.exp(-jnp.exp(w))
    a = jax.nn.sigmoid((a0 + xa @ a_up).astype(f32))
    g = jax.nn.sigmoid(xg) @ g_up
    hs = lambda t: t.astype(f32).reshape(B, T, RWKV_HEADS, RWKV_HEAD_DIM)
    hp = lambda t: t.astype(f32).reshape(RWKV_HEADS, RWKV_HEAD_DIM)
    kk = hs(k * k_k)
    kk = kk / jnp.maximum(jnp.sqrt(jnp.sum(kk * kk, -1, keepdims=True)), 1e-12)
    a = hs(a)
    k = hs(k) * (1.0 + (a - 1.0) * hp(k_a))
    r, v = hs(r), hs(v)
    y = rwkv7_scan(r, hs(decay), k, v, -kk, kk * a)
    mean = jnp.mean(y, -1, keepdims=True)
    var = jnp.mean(jnp.square(y - mean), -1, keepdims=True)
    y = (y - mean) * lax.rsqrt(var + RWKV_LN_EPS)
    y = y * hp(ln_w) + hp(ln_b)
    y = y + jnp.sum(r * k * r_k.astype(f32), -1, keepdims=True) * v
    return (y.reshape(B, T, D_RWKV) * g.astype(f32)).astype(z.dtype)


def memory_cross_attention(u, m, wq, wkv, wo):
    B, T, D = u.shape
    M = m.shape[1]
    q = (u @ wq).reshape(B, T, XATTN_HEADS, XATTN_HEAD_DIM)
    kv = m @ wkv
    k = kv[..., :D].reshape(B, M, XATTN_HEADS, XATTN_HEAD_DIM)
    v = kv[..., D:].reshape(B, M, XATTN_HEADS, XATTN_HEAD_DIM)
    s = jnp.einsum('bthd,bmhd->bhtm', q, k).astype(jnp.float32) * (XATTN_HEAD_DIM ** -0.5)
    p = jax.nn.softmax(s, -1).astype(v.dtype)
    o = jnp.einsum('bhtm,bmhd->bthd', p, v).reshape(B, T, D)
    return o @ wo


def setup_inputs(seed: int = 0) -> dict:
    key = jax.random.key(seed)
    ks = jax.random.split(key, 32)
    f32 = jnp.float32
    nrm = lambda k, shape, s: jax.random.normal(k, shape, f32) * s
    L_ = DEPTH
    ramp_w0 = jnp.linspace(-6.0, -1.0, D_RWKV, dtype=f32)
    ramp_fb = jnp.linspace(3.0, 6.0, MLSTM_HEADS, dtype=f32)
    return {
        'x': nrm(ks[0], (BATCH, SEQ, D_MODEL), 1.0),
        'mem': nrm(ks[1], (BATCH, MEM_TOKENS, D_MODEL), 1.0),
        'norm_mix': 1.0 + nrm(ks[2], (L_, D_MODEL), 0.02),
        'w_in': nrm(ks[3], (L_, D_MODEL, D_IN), D_MODEL ** -0.5),
        'mlstm_conv': nrm(ks[4], (L_, CONV_WIDTH, 2 * D_MLSTM), CONV_WIDTH ** -0.5),
        'mlstm_i_bias': nrm(ks[5], (L_, MLSTM_HEADS), 0.1),
        'mlstm_f_bias': ramp_fb + nrm(ks[6], (L_, MLSTM_HEADS), 0.1),
        'mlstm_norm': 1.0 + nrm(ks[7], (L_, D_MLSTM), 0.02),
        'rwkv_mu': jax.random.uniform(ks[8], (L_, D_IN_RWKV), f32),
        'rwkv_w0': ramp_w0 + nrm(ks[9], (L_, D_RWKV), 0.1),
        'rwkv_w_up': nrm(ks[10], (L_, DECAY_LORA, D_RWKV), 0.3 * DECAY_LORA ** -0.5),
        'rwkv_a0': nrm(ks[11], (L_, D_RWKV), 0.1),
        'rwkv_a_up': nrm(ks[12], (L_, ICLR_LORA, D_RWKV), ICLR_LORA ** -0.5),
        'rwkv_g_up': nrm(ks[13], (L_, GATE_LORA, D_RWKV), GATE_LORA ** -0.5),
        'rwkv_k_k': 0.85 + nrm(ks[14], (L_, D_RWKV), 0.05),
        'rwkv_k_a': 1.0 + nrm(ks[15], (L_, D_RWKV), 0.05),
        'rwkv_r_k': nrm(ks[16], (L_, RWKV_HEADS, RWKV_HEAD_DIM), 0.1),
        'rwkv_ln_w': 1.0 + nrm(ks[17], (L_, D_RWKV), 0.02),
        'rwkv_ln_b': nrm(ks[18], (L_, D_RWKV), 0.02),
        'w_mix_out': nrm(ks[19], (L_, D_MIX, D_MODEL), D_MIX ** -0.5),
        'norm_xattn': 1.0 + nrm(ks[20], (L_, D_MODEL), 0.02),
        'norm_mem': 1.0 + nrm(ks[21], (L_, D_MODEL), 0.02),
        'xattn_wq': nrm(ks[22], (L_, D_MODEL, D_MODEL), D_MODEL ** -0.5),
        'xattn_wkv': nrm(ks[23], (L_, D_MODEL, 2 * D_MODEL), D_MODEL ** -0.5),
        'xattn_wo': nrm(ks[24], (L_, D_MODEL, D_MODEL), D_MODEL ** -0.5),
        'norm_ffn': 1.0 + nrm(ks[25], (L_, D_MODEL), 0.02),
        'ffn_w_gate': nrm(ks[26], (L_, D_MODEL, D_FF), D_MODEL ** -0.5),
        'ffn_w_up': nrm(ks[27], (L_, D_MODEL, D_FF), D_MODEL ** -0.5),
        'ffn_w_down': nrm(ks[28], (L_, D_FF, D_MODEL), D_FF ** -0.5),
        'norm_final': 1.0 + nrm(ks[29], (D_MODEL,), 0.02),
    }


def reference(x, mem, norm_mix, w_in, mlstm_conv, mlstm_i_bias, mlstm_f_bias, mlstm_norm, rwkv_mu, rwkv_w0, rwkv_w_up, rwkv_a0, rwkv_a_up, rwkv_g_up, rwkv_k_k, rwkv_k_a, rwkv_r_k, rwkv_ln_w, rwkv_ln_b, w_mix_out, norm_xattn, norm_mem, xattn_wq, xattn_wkv, xattn_wo, norm_ffn, ffn_w_gate, ffn_w_up, ffn_w_down, norm_final):
    h = x
    for l in range(DEPTH):
        u = rms_norm(h, norm_mix[l])
        z = u @ w_in[l]
        y_m = mlstm_group(z[..., :D_IN_MLSTM], mlstm_conv[l], mlstm_i_bias[l], mlstm_f_bias[l], mlstm_norm[l])
        y_r = rwkv7_group(z[..., D_IN_MLSTM:], rwkv_mu[l], rwkv_w0[l], rwkv_w_up[l], rwkv_a0[l], rwkv_a_up[l],
                          rwkv_g_up[l], rwkv_k_k[l], rwkv_k_a[l], rwkv_r_k[l], rwkv_ln_w[l], rwkv_ln_b[l])
        h = h + jnp.concatenate([y_m, y_r], axis=-1) @ w_mix_out[l]
        h = h + memory_cross_attention(rms_norm(h, norm_xattn[l]), rms_norm(mem, norm_mem[l]),
                                       xattn_wq[l], xattn_wkv[l], xattn_wo[l])
        u = rms_norm(h, norm_ffn[l])
        h = h + (jax.nn.silu(u @ ffn_w_gate[l]) * (u @ ffn_w_up[l])) @ ffn_w_down[l]
    return rms_norm(h, norm_final)
```

```python
import contextlib
import numpy as np
import concourse.bass as bass
import concourse.mybir as mybir
from concourse.bass_utils import run_bass_kernel_spmd

F32 = mybir.dt.float32
BF16 = mybir.dt.bfloat16
ALU = mybir.AluOpType
AF = mybir.ActivationFunctionType
AX = mybir.AxisListType

class Prog:
    ENG = ("pe", "act", "dve", "pool", "sp")

    def __init__(self):
        self.ops = {e: [] for e in self.ENG}
        self.known = {e: {} for e in self.ENG}
        self.reg = {}
        self.used = set()
        self.dma_rr = {}
        self.dma_cnt = {}
        self.ndma_sems = 8

    def _subs(self, key):
        name, sub = key
        d = self.reg.setdefault(name, {})
        if "*" not in d:
            d["*"] = [None, {}]
        if sub is None:
            return [d[k] for k in d]
        if sub not in d:
            star = d["*"]
            d[sub] = [star[0], dict(star[1])]
        return [d[sub]]

    def _deps(self, reads, writes):
        deps = {}

        def add(evt):
            if evt is None:
                return
            s, i = evt
            if deps.get(s, 0) < i:
                deps[s] = i

        for k in reads:
            for st in self._subs(k):
                add(st[0])
        for k in writes:
            for st in self._subs(k):
                add(st[0])
                for s, i in st[1].items():
                    add((s, i))
        return deps

    def _register(self, evt, reads, writes):
        s, i = evt
        for k in reads:
            for st in self._subs(k):
                if st[1].get(s, 0) < i:
                    st[1][s] = i
        for k in writes:
            for st in self._subs(k):
                st[0] = evt
                st[1] = {}

    def add(self, eng, fn, reads=(), writes=(), dma=False, dma_sem=None):
        reads = list(reads)
        writes = list(writes)
        deps = self._deps(reads, writes)
        kn = self.known[eng]
        own = "E_" + eng
        waits = []
        if dma:
            rr = self.dma_rr.get(eng, 0)
            self.dma_rr[eng] = rr + 1
            sem = dma_sem or ("D_%s_%d" % (eng, rr % self.ndma_sems))
            n = self.dma_cnt.get(sem, 0)
            if n > 0 and dma_sem is None:
                deps[sem] = max(deps.get(sem, 0), n)
            self.dma_cnt[sem] = n + 1
            evt = (sem, n + 1)
        else:
            evt = (own, len(self.ops[eng]) + 1)
        for s, i in deps.items():
            if eng == "pe" and s == own:
                continue
            if kn.get(s, 0) >= i:
                continue
            kn[s] = i
            waits.append((s, i))
            self.used.add((s, i))
        if dma:
            self.used.add(evt)
        self.ops[eng].append(dict(fn=fn, waits=waits, evt=evt, dma=dma))
        self._register(evt, reads, writes)
        return evt

    def wait_all(self, eng, keys):
        self.add(eng, None, reads=keys, writes=())

    def emit(self, nc):
        semnames = set()
        for e in self.ENG:
            semnames.add("E_" + e)
        for s in self.dma_cnt:
            semnames.add(s)
        ranks = {}
        for e in self.ENG:
            own = "E_" + e
            idxs = sorted(i for (s, i) in self.used if s == own)
            ranks[own] = {i: r + 1 for r, i in enumerate(idxs)}
        with contextlib.ExitStack() as st:
            sems = {n: st.enter_context(nc.semaphore(n)) for n in sorted(semnames)}
            block = st.enter_context(nc.Block())

            def val(s, i):
                return ranks[s][i] if s.startswith("E_") else 16 * i

            def run(e, h):
                for op in self.ops[e]:
                    waits = [(sems[s], val(s, i)) for (s, i) in op["waits"]]
                    if op["fn"] is None:
                        for sh, v in waits:
                            h.wait_ge(sh, v)
                        continue
                    for sh, v in waits[1:]:
                        h.wait_ge(sh, v)
                    ins = op["fn"](h)
                    if waits:
                        ins._wait_ge(waits[0][0], waits[0][1])
                    evt = op["evt"]
                    if op["dma"]:
                        ins.then_inc(sems[evt[0]], 16)
                    elif evt in self.used:
                        ins.then_inc(sems[evt[0]], 1)

            @block.tensor
            def _(h):
                run("pe", h)

            @block.scalar
            def _(h):
                run("act", h)

            @block.vector
            def _(h):
                run("dve", h)

            @block.gpsimd
            def _(h):
                run("pool", h)

            @block.sync
            def _(h):
                run("sp", h)

    def count(self):
        return {e: len(v) for e, v in self.ops.items()}

    def barrier(self):
        last = {}
        for e in self.ENG:
            n = 0
            for i, op in enumerate(self.ops[e]):
                if op["fn"] is not None and not op["dma"]:
                    n = i + 1
            if n:
                last["E_" + e] = n
        for s, n in self.dma_cnt.items():
            last[s] = n
        for e in self.ENG:
            waits = []
            for s, i in last.items():
                if self.known[e].get(s, 0) >= i:
                    continue
                self.known[e][s] = i
                waits.append((s, i))
                self.used.add((s, i))
            self.ops[e].append(dict(fn=None, waits=waits, evt=("E_" + e, len(self.ops[e]) + 1), dma=False))
        self.reg = {}

D = 1024
KT = 8
TT = 256
NS = 2
DIN = 3848
DFF = 2816
NFF = 22
H = 16
MEMT = 256
EPS = 1e-6
OFF_V, OFF_O, OFF_IG, OFF_FG, OFF_R = 1024, 1536, 2048, 2052, 2056
RW = [(OFF_R + 128 * j, 128) for j in range(12)] + [(OFF_R + 1536, 64), (OFF_R + 1600, 64), (OFF_R + 1664, 128)]
DEC = 0.6065306597126334
LM = 128
LR = 64
NCM = TT // LM
NCR = TT // LR
WEIGHTS = {"w_in": (D, DIN), "w_up": (64, 512), "a_up": (64, 512), "g_up": (128, 512), "w_mix": (D, D),
           "wq": (D, D), "wkv": (D, 2 * D), "wo": (D, D), "wg": (D, DFF), "wu": (D, DFF), "wd": (DFF, D)}
SMALL = {"gains": (128, 4, KT), "convw": (128, 8, 4), "gb": (4, 2), "nw_bc": (128, 512), "mu15": (128, 15),
         "pp": (128, 6, 4), "ln_bc": (128, 2, 512), "nf_bc": (128, D), "ident": (128, 128), "maskU": (128, 128),
         "mXA": (128, 2, 128), "mL": (128, 128), "sel": (4, 8, 128), "mask4": (4, TT), "mask64": (128, TT),
         "blockones": (128, 128), "hsel": (128, 4, 8)}


class K:
    def __init__(self, nseq, T, dbg=None, phases="ABC"):
        self.nseq, self.T, self.dbg, self.phases = nseq, T, dbg, phases
        self.TT, self.NS = TT, NS
        self.nc = bass.Bass("TRN2", target_bir_lowering=False)
        self.P = Prog()
        self.st = contextlib.ExitStack()
        self.t = {}

    def sb(self, name, shape, dt=F32):
        return self.st.enter_context(self.nc.sbuf_tensor(name, list(shape), dt))

    def ps(self, name, shape, dt=F32):
        return self.st.enter_context(self.nc.psum_tensor(name, list(shape), dt))

    @staticmethod
    def _shape(v, shape):
        if len(shape) == 2:
            return v
        if len(shape) == 3:
            return v.rearrange("p (a b) -> p a b", a=shape[1])
        if len(shape) == 4:
            return v.rearrange("p (a b c) -> p a b c", a=shape[1], b=shape[2])
        raise ValueError(shape)

    def ab(self, name, shape):
        n = int(np.prod(shape[1:]))
        off = self.b_off
        self.b_off += (n + 7) // 8 * 8
        assert self.b_off <= self.NB, ("arena overflow", name, self.b_off)
        v = self._shape(self.arena_b[:, off:off + n], shape)
        self.t[name] = v
        return v

    def af(self, name, shape):
        n = int(np.prod(shape[1:]))
        off = self.b_off
        self.b_off += (2 * n + 7) // 8 * 8
        assert self.b_off <= self.NB, ("arena overflow", name, self.b_off)
        v = self._shape(self.arena_b[:, off:off + 2 * n].bitcast(F32), shape)
        self.t[name] = v
        return v

    def mm(self, out, lhsT, rhs, start, stop, r, w):
        self.P.add("pe", lambda h: h.matmul(out, lhsT, rhs, start=start, stop=stop), r, w)

    def tr(self, out, in_, ident, r, w):
        self.P.add("pe", lambda h: h.transpose(out, in_, ident), r, w)

    def act(self, out, in_, func, r, w, bias=0.0, scale=1.0, accum=None):
        if accum is None:
            self.P.add("act", lambda h: h.activation(out, in_, func, bias=bias, scale=scale), r, w)
        else:
            self.P.add("act", lambda h: h.activation(out, in_, func, bias=bias, scale=scale,
                                                     accum_out=accum), r, w)

    def ts(self, eng, out, in0, s1, s2, op0, op1, r, w):
        if op1 is None:
            self.P.add(eng, lambda h: h.tensor_scalar(out, in0, s1, None, op0), r, w)
        else:
            self.P.add(eng, lambda h: h.tensor_scalar(out, in0, s1, s2, op0, op1), r, w)

    def tt(self, eng, out, in0, in1, op, r, w):
        self.P.add(eng, lambda h: h.tensor_tensor(out, in0, in1, op), r, w)

    def stt(self, eng, out, in0, scalar, in1, op0, op1, r, w):
        self.P.add(eng, lambda h: h.scalar_tensor_tensor(out, in0, scalar, in1, op0, op1), r, w)

    def cp(self, eng, out, in_, r, w):
        if eng == "act":
            self.P.add("act", lambda h: h.copy(out, in_), r, w)
        else:
            self.P.add(eng, lambda h: h.tensor_copy(out, in_), r, w)

    def ms(self, eng, out, val, w):
        self.P.add(eng, lambda h: h.memset(out, val), [], w)

    def dma(self, q, out, in_, r, w):
        self.P.add(q, lambda h: h.dma_start(out=out, in_=in_), r, w, dma=True)

    def rsqrt_small(self, out, in_, mul, add, kout, kin):
        self.ts("dve", out, in_, float(mul), float(add), ALU.mult, ALU.add, [kin], [kout])
        self.act(out, out, AF.Ln, [kout], [kout])
        self.act(out, out, AF.Exp, [kout], [kout], scale=-0.5)

    def nps(self):
        i = self.ps_rr % 5
        self.ps_rr += 1
        return self.psf[i], ("psf%d" % i, None)

    def npt(self):
        i = self.pt_rr % len(self.pst)
        self.pt_rr += 1
        return self.pst[i], ("pst%d" % i, None)

    def dq(self):
        self.dq_rr += 1
        return "sp" if self.dq_rr % 2 else "act"

    def build(self):
        nseq, T = self.nseq, self.T
        NTOK = nseq * T
        nc = self.nc
        di = lambda n, s, dt=F32: nc.dram_tensor(n, list(s), dt, kind="ExternalInput").ap()
        self.x = di("x", [NTOK, D])
        self.mem = di("mem", [nseq * MEMT, D])
        self.wsrc = {k: di(k, v) for k, v in WEIGHTS.items()}
        self.ssrc = {k: di(k, v) for k, v in SMALL.items()}
        self.out = nc.dram_tensor("out", [NTOK, D], F32, kind="ExternalOutput").ap()
        self.wbf = {k: nc.dram_tensor("bf_" + k, list(v), BF16, kind="Internal").ap() for k, v in WEIGHTS.items()}
        self.h1d = nc.dram_tensor("h1d", [NTOK, D], F32, kind="Internal").ap()
        self.h2d = nc.dram_tensor("h2d", [NTOK, D], F32, kind="Internal").ap()
        if self.dbg:
            self.dbgd = {k: nc.dram_tensor("dbg_" + k, list(s), F32, kind="ExternalOutput").ap()
                         for k, s in self.dbg.items()}

        self.NB = 104000
        self.arena_b = self.sb("arena_b", [128, self.NB], BF16)
        self.psf = [self.ps("psf%d" % i, [128, 512]) for i in range(6)]
        self.pst = [self.ps("pst%d" % i, [128, 1024], BF16) for i in range(2)]
        self.ps_rr = self.pt_rr = self.dq_rr = 0
        self.b_off = self.f_off = 0

        wk = []
        for k, (rows, cols) in WEIGHTS.items():
            for r0 in range(0, rows, 128):
                r1 = min(rows, r0 + 128)
                self.P.add("pool", lambda h, o=self.wbf[k][r0:r1, :], i=self.wsrc[k][r0:r1, :]: h.dma_start(out=o, in_=i),
                           [], [("wbf_" + k, r0)], dma=True, dma_sem="DW_" + k)
            wk.append(("wbf_" + k, None))
        scr = self.af("scr", [128, 4])
        self.P.add("pool", lambda h: h.memset(scr[:, 1:2], 0.0), [], [("scr", 1)])
        self.P.add("dve", lambda h: h.memset(scr[:, 0:1], 0.0), [("scr", 1)], [("scr", 0)])

        if "A" in self.phases:
            self.phaseA()
        self.P.barrier()
        self.b_off = 0
        if "B" in self.phases:
            self.phaseB()
        self.P.barrier()
        self.b_off = 0
        if "C" in self.phases:
            self.phaseC()
        self.P.wait_all("sp", [("out_d", None)] + [("dbg_" + k, None) for k in (self.dbg or {})])
        self.P.emit(nc)
        self.st.close()
        return nc

    def load_consts(self, names):
        for k in names:
            shp = SMALL[k]
            v = self.af("c_" + k, [128] + list(shp[1:]))
            self.dma(self.dq(), v[0:shp[0]], self.ssrc[k], [], [("c_" + k, None)])

    def consts_bf(self, names):
        for k in names:
            shp = SMALL[k]
            v = self.ab("b_" + k, [128] + list(shp[1:]))
            self.cp("act", v[0:shp[0]], self.t["c_" + k][0:shp[0]], [("c_" + k, None)], [("b_" + k, None)])

    def load_w(self, key, name, rows, cols):
        if rows <= 128:
            v = self.ab(name, [128, cols])
            self.dma(self.dq(), v[0:rows, :], self.wbf[key], [("wbf_" + key, None)], [(name, None)])
            return v
        kt = rows // 128
        v = self.ab(name, [128, kt, cols])
        src = self.wbf[key].rearrange("(k p) n -> p k n", p=128)
        step = max(1, kt // 4)
        for k0 in range(0, kt, step):
            k1 = min(kt, k0 + step)
            self.dma(self.dq(), v[:, k0:k1, :], src[:, k0:k1, :], [("wbf_" + key, None)], [(name, (k0,))])
        return v

    def dbg_dump(self, key, dst_ap, src_ap, rkeys):
        if self.dbg and key in self.dbg:
            self.dma("sp", dst_ap, src_ap, rkeys, [("dbg_" + key, None)])

    def norm_T(self, gi):
        t = self.t
        NS_ = self.NS
        ht, ssq, rstd, junk, uT, gains, ident = t["ht"], t["ssq"], t["rstd"], t["junk"], t["uT"], t["c_gains"], t["b_ident"]
        for s in range(NS_):
            self.act(junk[:], ht[:, s, :], AF.Square, [("ht", s)], [("junk", None), ("ssq", s)],
                     accum=ssq[:, s:s + 1])
        self.rsqrt_small(rstd[:, 0:NS_], ssq[:, 0:NS_], 1.0 / D, EPS, ("rstd", None), ("ssq", None))
        for s in range(NS_):
            xs = t["xs%d" % (s % 2)]
            xk = ("xs%d" % (s % 2), None)
            self.act(xs[:], ht[:, s, :], AF.Copy, [("ht", s), ("rstd", None)], [xk], scale=rstd[:, s:s + 1])
            for half in range(2):
                pt, pk = self.npt()
                for j in range(4):
                    k = half * 4 + j
                    self.tr(pt[:, j * 128:(j + 1) * 128], xs[:, k * 128:(k + 1) * 128], ident[:],
                            [xk, ("b_ident", None)], [pk])
                for j in range(4):
                    k = half * 4 + j
                    o = uT[:, k, H + s * 128:H + (s + 1) * 128]
                    if False:
                        self.act(o, pt[:, j * 128:(j + 1) * 128], AF.Copy, [pk, ("c_gains", None)], [("uT", (k, s))],
                                 scale=gains[:, gi, k:k + 1])
                    else:
                        self.ts("dve", o, pt[:, j * 128:(j + 1) * 128], gains[:, gi, k:k + 1], None, ALU.mult, None,
                                [pk, ("c_gains", None)], [("uT", (k, s))])

    def proj_fm(self, wname, w, c0, M, ps, pk, h0=H):
        TT_ = self.TT
        for k in range(KT):
            self.mm(ps[0:M, 0:TT_ + H - h0], w[:, k, c0:c0 + M], self.t["uT"][:, k, h0:TT_ + H], k == 0, k == KT - 1,
                    [(wname, None), ("uT", None)], [pk])

    def proj_tm(self, wname, w, nk, lhs, lname, s, c0, N, ps, pk, off=0):
        for k in range(nk):
            self.mm(ps[:, 0:N], lhs[:, k, off + s * 128:off + (s + 1) * 128], w[:, k, c0:c0 + N], k == 0, k == nk - 1,
                    [(wname, None), (lname, None)], [pk])

    def phaseA(self):
        t = self.t
        self.load_consts(["gains", "convw", "gb", "nw_bc", "mu15", "pp", "ln_bc", "ident", "maskU", "mXA", "mL", "sel",
                          "mask4", "mask64", "blockones", "hsel"])
        self.consts_bf(["ident", "blockones", "hsel"])
        id4 = self.ab("id4", [128, 4, 128])
        for i in range(4):
            self.cp("act", id4[:, i, :], t["c_ident"][:], [("c_ident", None)], [("id4", i)])
        self.load_w("w_in", "w_in", D, DIN)
        self.load_w("w_mix", "w_mix", D, D)
        self.load_w("w_up", "w_up", 64, 512)
        self.load_w("a_up", "a_up", 64, 512)
        self.load_w("g_up", "g_up", 128, 512)
        self.af("ht", [128, NS, D])
        self.af("ssq", [128, NS])
        self.af("rstd", [128, NS])
        self.ab("junk", [128, D])
        self.ab("xs0", [128, D])
        self.ab("xs1", [128, D])
        self.ab("uT", [128, KT, TT + H])
        self.ab("yT", [128, KT, TT])
        C32 = self.af("C32", [128, 4, 129])
        Cb = self.ab("Cb", [128, 4, 129])
        S32 = self.af("S32", [128, 4, 128])
        Sb = self.ab("Sb", [128, 4, 128])
        nfb = self.af("nfb", [128, 1])
        omka = self.af("omka", [128, 4])
        v_aug = self.ab("v_aug", [128, NCM, 4, 129])
        Bbd = self.ab("Bbd", [128, 4, NCR, 128])
        Kbd = self.ab("Kbd", [128, 4, NCR, 128])
        ARbd = self.ab("ARbd", [128, 4, NCR, 256])
        Vbdt = self.ab("Vbdt", [128, 4, NCR, 128])
        self.ts("dve", nfb[0:4, :], t["c_gb"][0:4, 1:2], -1.0, None, ALU.mult, None, [("c_gb", None)], [("nfb", None)])
        self.ts("dve", omka[:], t["c_pp"][:, 3, :], -1.0, 1.0, ALU.mult, ALU.add, [("c_pp", None)], [("omka", None)])
        self.ts("dve", t["c_nw_bc"][:], t["c_nw_bc"][:], 0.5, None, ALU.mult, None, [("c_nw_bc", None)], [("c_nw_bc", None)])
        self.ms("dve", v_aug[:], 1.0, [("v_aug", None)])
        for nm in ("Bbd", "Kbd", "ARbd", "Vbdt"):
            self.ms("dve", t[nm][:], 0.0, [(nm, None)])
        self.markA = self.b_off
        for b in range(self.nseq):
            self.ms("dve", C32[:], 0.0, [("C32", None)])
            self.ms("dve", Cb[:], 0.0, [("Cb", None)])
            self.ms("dve", S32[:], 0.0, [("S32", None)])
            self.ms("dve", Sb[:], 0.0, [("Sb", None)])
            self.ms("dve", t["uT"][:, :, 0:H], 0.0, [("uT", "h")])
            for ti in range(self.T // TT):
                self.tileA(b, ti)

    def tileA(self, b, ti):
        t = self.t
        r0 = b * self.T + ti * TT
        self.b_off = self.markA
        xv = self.x[r0:r0 + TT, :].rearrange("(s p) d -> p s d", p=128)
        self.dma("sp", t["ht"][:], xv, [], [("ht", None)])
        if ti > 0:
            self.cp("act", t["uT"][:, :, 0:H], t["uT"][:, :, TT:TT + H], [("uT", None)], [("uT", "h")])
        self.norm_T(0)
        self.P.barrier()
        self.mlstm()
        self.P.barrier()
        self.b_off = self.markA
        self.rwkv()
        self.b_off = self.markA
        if self.dbg and "yT" in self.dbg:
            for k in range(KT):
                yd = self.af("yd%d" % k, [128, TT])
                self.cp("act", yd[:], t["yT"][:, k, :], [("yT", None)], [("yd%d" % k, None)])
                self.dma("sp", self.dbgd["yT"][k * 128:(k + 1) * 128, r0:r0 + TT], yd[:], [("yd%d" % k, None)],
                         [("dbg_yT", (k, r0))])
        for s in range(NS):
            for half in range(2):
                ps, pk = self.nps()
                self.proj_tm("w_mix", t["w_mix"], KT, t["yT"], "yT", s, half * 512, 512, ps, pk)
                hv = t["ht"][:, s, half * 512:(half + 1) * 512]
                self.tt("dve", hv, hv, ps[:, :], ALU.add, [pk, ("ht", s)], [("ht", s)])
        hd = self.h1d[r0:r0 + TT, :].rearrange("(s p) d -> p s d", p=128)
        self.dma("sp", hd, t["ht"][:], [("ht", None)], [("h1d", (b, ti))])

    def mlstm(self):
        t = self.t
        w_in, uT, ident = t["w_in"], t["uT"], t["b_ident"]
        convw, gb, nfb = t["c_convw"], t["c_gb"], t["nfb"]
        C32, Cb, v_aug, yT = t["C32"], t["Cb"], t["v_aug"], t["yT"]
        qk = self.ab("qk", [128, 8, TT])
        qkt = self.ab("qkt", [128, 8, TT])
        ktok = self.ab("ktok", [128, 4, NCM, 128])
        gate = self.ab("gate", [128, NS, 512])
        PT = self.ab("PT", [128, 4, NCM, 128])
        ymt = [self.ab("ym%d" % i, [128, 512]) for i in range(2)]
        zt = [self.af("zt%d" % i, [128, TT + H]) for i in range(2)]
        acc = [self.af("acc%d" % i, [128, TT]) for i in range(2)]
        so = self.af("so", [128, 512])
        ig = self.af("ig", [128, TT])
        e1 = self.af("e1", [128, TT])
        bneg = self.af("bneg", [128, TT])
        eb = self.af("eb", [128, TT])
        ek = self.af("ek", [128, TT])
        eg = self.af("eg", [128, 4, NCM])
        Cegs = [self.af("Ceg%d" % i, [128, 2, 129]) for i in range(2)]
        dds = [self.af("dd%d" % i, [128, 2]) for i in range(2)]
        ss2s = [self.af("ss2%d" % i, [128, 2]) for i in range(2)]
        hns = [self.af("hn%d" % i, [128, 2, 128]) for i in range(2)]
        psi, pki = self.nps()
        self.proj_fm("w_in", w_in, OFF_IG, 4, psi, pki)
        psg, pkg = self.nps()
        self.proj_fm("w_in", w_in, OFF_FG, 4, psg, pkg)
        self.act(ig[0:4, :], psi[0:4, 0:TT], AF.Identity, [pki, ("c_gb", None)], [("ig", None)], bias=gb[0:4, 0:1])
        self.act(e1[0:4, :], psg[0:4, 0:TT], AF.Exp, [pkg, ("nfb", None)], [("e1", None)], bias=nfb[0:4, 0:1], scale=-1.0)
        self.act(e1[0:4, :], e1[0:4, :], AF.Ln, [("e1", None)], [("e1", None)], bias=1.0)
        m4 = t["c_mask4"]
        self.P.add("dve", lambda h: h.tensor_tensor_scan(bneg[0:4, :], m4[0:4, :], e1[0:4, :], 0.0, ALU.mult, ALU.add),
                   [("e1", None), ("c_mask4", None)], [("bneg", None)])
        self.act(eb[0:4, :], bneg[0:4, :], AF.Exp, [("bneg", None)], [("eb", None)], scale=-1.0)
        self.tt("dve", ek[0:4, :], ig[0:4, :], bneg[0:4, :], ALU.add, [("ig", None), ("bneg", None)], [("ek", None)])
        self.act(ek[0:4, :], ek[0:4, :], AF.Exp, [("ek", None)], [("ek", None)])
        for fp in range(4):
            grp = []
            for i in range(2):
                f = fp * 2 + i
                ps, pk = self.nps()
                self.proj_fm("w_in", w_in, f * 128, 128, ps, pk, h0=0)
                z, zk = zt[i], ("zt%d" % i, None)
                a, ak = acc[i], ("acc%d" % i, None)
                self.act(z[:, 0:TT + H], ps[:, 0:TT + H], AF.Copy, [pk], [zk])
                grp.append((f, z, zk, a, ak))
            for (f, z, zk, a, ak) in grp:
                self.ts("dve", a[:], z[:, H - 3:H - 3 + TT], convw[:, f, 0:1], None, ALU.mult, None, [zk, ("c_convw", None)], [ak])
            for j in range(1, 4):
                for (f, z, zk, a, ak) in grp:
                    self.stt("dve", a[:], z[:, H - 3 + j:H - 3 + j + TT], convw[:, f, j:j + 1], a[:], ALU.mult, ALU.add,
                             [zk, ak, ("c_convw", None)], [ak])
            for (f, z, zk, a, ak) in grp:
                self.act(qk[:, f, :], a[:], AF.Silu, [ak], [("qk", f)])
        for s in range(NS):
            ps, pk = self.nps()
            self.proj_tm("w_in", w_in, KT, uT, "uT", s, OFF_V, 512, ps, pk, off=H)
            self.act(v_aug[:, s, :, 0:128], ps[:, :].rearrange("p (h d) -> p h d", h=4), AF.Copy, [pk], [("v_aug", s)])
            ps, pk = self.nps()
            self.proj_tm("w_in", w_in, KT, uT, "uT", s, OFF_O, 512, ps, pk, off=H)
            self.act(so[:], ps[:, :], AF.Tanh, [pk], [("so", None)], scale=0.5)
            self.stt("dve", gate[:, s, :], so[:], 1.0, t["c_nw_bc"][:], ALU.add, ALU.mult, [("so", None), ("c_nw_bc", None)],
                     [("gate", s)])
        sel = t["c_sel"]
        for h in range(4):
            ps, pk = self.nps()
            self.mm(ps[:, 0:TT], sel[0:4, h, :], eb[0:4, :], True, True, [("c_sel", None), ("eb", None)], [pk])
            self.tt("dve", qkt[:, h, :], qk[:, h, :], ps[:, 0:TT], ALU.mult, [("qk", h), pk], [("qkt", h)])
            self.cp("act", eg[:, h, :], ps[:, 0:TT].rearrange("p (c t) -> p c t", t=LM)[:, :, LM - 1], [pk], [("eg", h)])
            ps2, pk2 = self.nps()
            self.mm(ps2[:, 0:TT], sel[0:4, 4 + h, :], ek[0:4, :], True, True, [("c_sel", None), ("ek", None)], [pk2])
            self.tt("dve", qkt[:, 4 + h, :], qk[:, 4 + h, :], ps2[:, 0:TT], ALU.mult, [("qk", 4 + h), pk2], [("qkt", 4 + h)])
        for h in range(4):
            pt, pk = self.npt()
            for c in range(NCM):
                self.tr(pt[:, c * 128:(c + 1) * 128], qkt[:, 4 + h, c * 128:(c + 1) * 128], ident[:],
                        [("qkt", 4 + h), ("b_ident", None)], [pk])
            self.cp("act", ktok[:, h, :, :], pt[:, 0:NCM * 128].rearrange("p (c e) -> p c e", c=NCM), [pk], [("ktok", h)])
            ps, pk = self.nps()
            for c in range(NCM):
                cs = slice(c * 128, (c + 1) * 128)
                self.mm(ps[:, cs], qkt[:, 4 + h, cs], qkt[:, h, cs], True, True, [("qkt", 4 + h), ("qkt", h)], [pk])
            for c in range(NCM):
                self.tt("dve", PT[:, h, c, :], ps[:, c * 128:(c + 1) * 128], t["c_maskU"][:], ALU.mult,
                        [pk, ("c_maskU", None)], [("PT", (h, c))])
        def hp_stream(c, hp, ym, ymk):
            cs = slice(c * 128, (c + 1) * 128)
            dd, ss2, hn, Ceg = dds[hp], ss2s[hp], hns[hp], Cegs[hp]
            kd, ks, kh, kc = "dd%d" % hp, "ss2%d" % hp, "hn%d" % hp, "Ceg%d" % hp
            psn, pkn = self.nps()
            pss, pks = self.nps()
            for j in range(2):
                h = hp * 2 + j
                js = slice(j * 129, (j + 1) * 129)
                self.mm(psn[:, js], PT[:, h, c, :], v_aug[:, c, h, :], True, False,
                        [("PT", (h, c)), ("v_aug", c)], [pkn])
                self.mm(psn[:, js], qkt[:, h, cs], Cb[:, h, :], False, True, [("qkt", h), ("Cb", h)], [pkn])
                self.mm(pss[:, js], ktok[:, h, c, :], v_aug[:, c, h, :], True, True,
                        [("ktok", h), ("v_aug", c)], [pks])
            yield
            nv = psn[:, 0:258].rearrange("p (j d) -> p j d", j=2)
            self.act(dd[:], nv[:, :, 128], AF.Abs, [pkn], [(kd, None)])
            for j in range(2):
                h = hp * 2 + j
                self.act(Ceg[:, j, :], C32[:, h, :], AF.Copy, [("C32", h), ("eg", h)], [(kc, j)],
                         scale=eg[:, h, c:c + 1])
            yield
            self.ts("dve", dd[:], dd[:], 1.0, None, ALU.max, None, [(kd, None)], [(kd, None)])
            for j in range(2):
                h = hp * 2 + j
                js = slice(j * 129, (j + 1) * 129)
                self.stt("dve", C32[:, h, :], pss[:, js], eg[:, h, c:c + 1], Ceg[:, j, :], ALU.mult, ALU.add,
                         [pks, ("eg", h), (kc, j)], [("C32", h)])
            yield
            self.P.add("dve", lambda h_, dd=dd: h_.reciprocal(dd[:], dd[:]), [(kd, None)], [(kd, None)])
            for j in range(2):
                h = hp * 2 + j
                self.cp("act", Cb[:, h, :], C32[:, h, :], [("C32", h)], [("Cb", h)])
            yield
            for j in range(2):
                self.ts("dve", hn[:, j, :], nv[:, j, 0:128], dd[:, j:j + 1], None, ALU.mult, None,
                        [pkn, (kd, None)], [(kh, j)])
            yield
            for j in range(2):
                jk = ("junk", (hp, j))
                self.act(t["junk"][:, (hp * 2 + j) * 128:(hp * 2 + j + 1) * 128], hn[:, j, :], AF.Square, [(kh, j)],
                         [jk, (ks, j)], accum=ss2[:, j:j + 1])
            yield
            self.ts("dve", ss2[:], ss2[:], 1.0 / 128, EPS, ALU.mult, ALU.add, [(ks, None)], [(ks, None)])
            yield
            self.act(ss2[:], ss2[:], AF.Ln, [(ks, None)], [(ks, None)])
            yield
            self.act(ss2[:], ss2[:], AF.Exp, [(ks, None)], [(ks, None)], scale=-0.5)
            yield
            for j in range(2):
                h = hp * 2 + j
                self.stt("dve", ym[:, h * 128:(h + 1) * 128], hn[:, j, :], ss2[:, j:j + 1],
                         gate[:, c, h * 128:(h + 1) * 128], ALU.mult, ALU.mult,
                         [(kh, j), (ks, None), ("gate", c)], [(ymk[0], h)])
            yield

        for c in range(NCM):
            cs = slice(c * 128, (c + 1) * 128)
            ym, ymk = ymt[c % 2], ("ym%d" % (c % 2), None)
            gens = [hp_stream(c, 0, ym, ymk), hp_stream(c, 1, ym, ymk)]
            while gens:
                for g in list(gens):
                    try:
                        next(g)
                    except StopIteration:
                        gens.remove(g)
            pt, pk = self.npt()
            for h in range(4):
                self.tr(pt[:, h * 128:(h + 1) * 128], ym[:, h * 128:(h + 1) * 128], ident[:], [ymk, ("b_ident", None)], [pk])
            self.cp("act", yT[:, 0:4, cs], pt[:, 0:512].rearrange("p (h t) -> p h t", h=4), [pk], [("yT", ("m", c))])

    def rwkv(self):
        t = self.t
        w_in, ident, id4 = t["w_in"], t["b_ident"], t["id4"]
        mu, pp, omka = t["c_mu15"], t["c_pp"], t["omka"]
        S32, Sb, yT = t["S32"], t["Sb"], t["yT"]
        Bbd, Kbd, ARbd, Vbdt = t["Bbd"], t["Kbd"], t["ARbd"], t["Vbdt"]
        mXA, mL = t["c_mXA"], t["c_mL"]
        txw = self.ab("txw", [128, TT])
        xab = self.ab("xab", [128, TT])
        sxg = self.ab("sxg", [128, TT])
        kq = self.ab("kq", [128, TT])
        prod = self.ab("prod", [128, 4, TT])
        rnat = self.ab("rnat", [128, 4, TT])
        vnat = self.ab("vnat", [128, 4, TT])
        Vbd = self.ab("Vbd", [128, NCR, 4, 128])
        zt = [self.af("rz%d" % i, [128, TT + 8]) for i in range(2)]
        dt_ = [self.af("rd%d" % i, [128, TT]) for i in range(2)]
        PL = self.af("PL", [128, 4, NCR])
        names = ["R32", "K32", "V32", "sg", "cs", "Ep", "Em", "Epv", "KK", "tq", "KM"]
        F = {n: self.af("r_" + n, [128, TT]) for n in names}
        FK = {n: ("r_" + n, None) for n in names}
        zrr = [0]

        def lerp(items):
            grp = []
            for (j, dest, dk) in items:
                off, M = RW[j]
                ps, pk = self.nps()
                self.proj_fm("w_in", w_in, off, M, ps, pk, h0=H - 8)
                i = zrr[0] % 2
                zrr[0] += 1
                z, zk, d, dk_ = zt[i], ("rz%d" % i, None), dt_[i], ("rd%d" % i, None)
                self.act(z[0:M, 0:TT + 8], ps[0:M, 0:TT + 8], AF.Copy, [pk], [zk])
                grp.append((j, M, dest, dk, z, zk, d, dk_))
            for (j, M, dest, dk, z, zk, d, dk_) in grp:
                self.tt("dve", d[0:M, :], z[0:M, 7:TT + 7], z[0:M, 8:TT + 8], ALU.subtract, [zk], [dk_])
            for (j, M, dest, dk, z, zk, d, dk_) in grp:
                self.stt("dve", dest[0:M, :], d[0:M, :], mu[0:M, j:j + 1], z[0:M, 8:TT + 8], ALU.mult, ALU.add,
                         [dk_, zk, ("c_mu15", None)], [dk])

        lerp([(14, F["tq"], FK["tq"])])
        self.act(sxg[:], F["tq"][:], AF.Sigmoid, [FK["tq"]], [("sxg", None)])
        lerp([(12, F["tq"], FK["tq"]), (13, F["KM"], FK["KM"])])
        self.act(txw[0:64, :], F["tq"][0:64, :], AF.Tanh, [FK["tq"]], [("txw", None)])
        self.cp("act", xab[0:64, :], F["KM"][0:64, :], [FK["KM"]], [("xab", None)])
        sgs = [F["sg"]] + [self.af("r_sg%d" % i, [128, TT]) for i in range(1, 4)]
        sgk = [FK["sg"]] + [("r_sg%d" % i, None) for i in range(1, 4)]
        A32s = [self.ab("r_A%d" % i, [128, TT]) for i in range(4)]
        A32k = [("r_A%d" % i, None) for i in range(4)]
        for f in range(4):
            fs = slice(f * 128, (f + 1) * 128)
            psw, pkw = self.nps()
            self.mm(psw[:, 0:TT], t["w_up"][0:64, fs], txw[0:64, :], True, True, [("w_up", None), ("txw", None)], [pkw])
            psa, pka = self.nps()
            self.mm(psa[:, 0:TT], t["a_up"][0:64, fs], xab[0:64, :], True, True, [("a_up", None), ("xab", None)], [pka])
            self.act(sgs[f][:], psw[:, 0:TT], AF.Sigmoid, [pkw, ("c_pp", None)], [sgk[f]], bias=pp[:, 0, f:f + 1])
            self.act(A32s[f][:], psa[:, 0:TT], AF.Sigmoid, [pka, ("c_pp", None)], [A32k[f]], bias=pp[:, 1, f:f + 1])

        c3 = lambda ap: ap.rearrange("p (c t) -> p c t", t=LR)
        for f in range(4):
            R32, K32, V32, sg, cs, Ep, Em, Epv = (F[n] for n in ["R32", "K32", "V32", "sg", "cs", "Ep", "Em", "Epv"])
            KK, tq, KM = (F[n] for n in ["KK", "tq", "KM"])
            sg, A32 = sgs[f], A32s[f]
            FK = dict(FK)
            FK["sg"], FK["A32"] = sgk[f], A32k[f]
            lerp([(f, R32, FK["R32"]), (4 + f, K32, FK["K32"])])
            lerp([(8 + f, V32, FK["V32"])])
            fs = slice(f * 128, (f + 1) * 128)
            m64 = t["c_mask64"]
            self.act(kq[:], K32[:], AF.Square, [FK["K32"], ("c_pp", None)], [("kq", None)], scale=pp[:, 2, f:f + 1])
            pss_, pks_ = self.nps()
            self.mm(pss_[:, 0:TT], t["b_blockones"][:], kq[:], True, True, [("b_blockones", None), ("kq", None)], [pks_])
            self.ts("dve", tq[:], pss_[:, 0:TT], 1.0, 1e-24, ALU.mult, ALU.add, [pks_], [FK["tq"]])
            self.P.add("dve", lambda h, cs=cs, sg=sg, m64=m64: h.tensor_tensor_scan(cs[:], m64[:], sg[:], 0.0, ALU.mult, ALU.add),
                       [FK["sg"], ("c_mask64", None)], [FK["cs"]])
            self.act(tq[:], tq[:], AF.Ln, [FK["tq"]], [FK["tq"]])
            self.act(Ep[:], cs[:], AF.Exp, [FK["cs"]], [FK["Ep"]], scale=-DEC)
            self.act(tq[:], tq[:], AF.Exp, [FK["tq"]], [FK["tq"]], scale=-0.5)
            self.act(Em[:], cs[:], AF.Exp, [FK["cs"]], [FK["Em"]], scale=DEC)
            self.tt("dve", sg[:], cs[:], sg[:], ALU.subtract, [FK["cs"], FK["sg"]], [FK["sg"]])
            self.stt("dve", KK[:], K32[:], pp[:, 2, f:f + 1], tq[:], ALU.mult, ALU.mult,
                     [FK["K32"], FK["tq"], ("c_pp", None)], [FK["KK"]])
            self.act(Epv[:], sg[:], AF.Exp, [FK["sg"]], [FK["Epv"]], scale=-DEC)
            self.cp("act", PL[:, f, :], c3(Ep[:])[:, :, LR - 1], [FK["Ep"]], [("PL", f)])
            self.tt("dve", rnat[:, f, :], R32[:], Ep[:], ALU.mult, [FK["R32"], FK["Ep"]], [("rnat", f)])
            self.ts("dve", tq[:], A32[:], pp[:, 3, f:f + 1], omka[:, f:f + 1], ALU.mult, ALU.add,
                    [FK["A32"], ("c_pp", None), ("omka", None)], [FK["tq"]])
            self.cp("act", vnat[:, f, :], V32[:], [FK["V32"]], [("vnat", f)])
            self.tt("dve", KM[:], K32[:], tq[:], ALU.mult, [FK["K32"], FK["tq"]], [FK["KM"]])
            self.tt("dve", tq[:], KK[:], A32[:], ALU.mult, [FK["KK"], FK["A32"]], [FK["tq"]])
            self.stt("dve", prod[:, f, :], R32[:], pp[:, 4, f:f + 1], KM[:], ALU.mult, ALU.mult,
                     [FK["R32"], FK["KM"], ("c_pp", None)], [("prod", f)])
            for hh in range(2):
                rs = slice(hh * 64, hh * 64 + 64)
                cols = slice(hh * 64, hh * 64 + 64)
                cols_r = slice(128 + hh * 64, 128 + hh * 64 + 64)
                self.stt("dve", ARbd[rs, f, :, cols], c3(KK[rs, :]), -1.0, c3(Epv[rs, :]), ALU.mult, ALU.mult,
                         [FK["KK"], FK["Epv"]], [("ARbd", (f, "a", hh))])
                self.cp("act", ARbd[rs, f, :, cols_r], c3(rnat[rs, f, :]), [("rnat", f)], [("ARbd", (f, "r", hh))])
                self.tt("dve", Bbd[rs, f, :, cols], c3(tq[rs, :]), c3(Em[rs, :]), ALU.mult, [FK["tq"], FK["Em"]],
                        [("Bbd", (f, hh))])
                self.tt("dve", Kbd[rs, f, :, cols], c3(KM[rs, :]), c3(Em[rs, :]), ALU.mult, [FK["KM"], FK["Em"]],
                        [("Kbd", (f, hh))])
                self.cp("act", Vbdt[rs, f, :, cols], c3(vnat[rs, f, :]), [("vnat", f)], [("Vbdt", (f, hh))])
            pt, pk = self.npt()
            for c in range(NCR):
                self.tr(pt[:, c * 128:(c + 1) * 128], Vbdt[:, f, c, :], ident[:], [("Vbdt", None), ("b_ident", None)], [pk])
            self.cp("act", Vbd[:, :, f, :], pt[:, 0:NCR * 128].rearrange("p (c i) -> p c i", c=NCR), [pk], [("Vbd", f)])

        XA = self.ab("XA", [128, 4, 2, 128])
        KA = self.ab("KA", [128, 4, 2, 128])
        Xp = [self.ab("Xp%d" % i, [128, 4, 128]) for i in range(2)]
        Np = [self.ab("Np%d" % i, [128, 4, 128]) for i in range(2)]
        Rm = [self.ab("Rm%d" % i, [128, 4, 128]) for i in range(2)]
        tmpR = self.ab("tmpR", [128, 4, 128])
        XAs = self.ab("XAs", [128, 4, 64])
        KAs = self.ab("KAs", [128, 4, 64])
        BK = self.ab("BKtok", [128, 2, 4, 128])
        Wb = self.ab("Wb", [128, 4, 128])
        Ub = self.ab("Ub", [128, 4, 128])
        yr = self.ab("yr", [128, 512])
        Y32 = self.af("Y32", [128, 512])
        sq32 = self.af("sq32", [128, 512])
        st1 = self.af("st1", [128, 8])
        st2 = self.af("st2", [128, 8])
        st3 = self.af("st3", [128, 8])
        bon = self.af("bon", [128, 8])
        tS = self.af("tS", [128, 4, 128])
        hp4 = lambda ap: ap.rearrange("p (a b) -> p a b", a=4)
        lnb = t["c_ln_bc"]
        h8 = lambda ap: ap.rearrange("p (h i) -> p h i", h=8)
        Rfin = {}

        def prep(c):
            for pair in range(2):
                ps, pk = self.nps()
                for j in range(2):
                    hp = pair * 2 + j
                    self.mm(ps[:, j * 256:(j + 1) * 256], Bbd[:, hp, c, :], ARbd[:, hp, c, :], True, True,
                            [("Bbd", None), ("ARbd", None)], [pk])
                for j in range(2):
                    hp = pair * 2 + j
                    self.tt("dve", XA[:, hp, :, :], ps[:, j * 256:(j + 1) * 256].rearrange("p (a b) -> p a b", a=2), mXA[:],
                            ALU.mult, [pk, ("c_mXA", None)], [("XA", hp)])
                ps, pk = self.nps()
                for j in range(2):
                    hp = pair * 2 + j
                    self.mm(ps[:, j * 256:(j + 1) * 256], Kbd[:, hp, c, :], ARbd[:, hp, c, :], True, True,
                            [("Kbd", None), ("ARbd", None)], [pk])
                for j in range(2):
                    hp = pair * 2 + j
                    self.tt("dve", KA[:, hp, :, :], ps[:, j * 256:(j + 1) * 256].rearrange("p (a b) -> p a b", a=2), mXA[:],
                            ALU.mult, [pk, ("c_mXA", None)], [("KA", hp)])
            ps, pk = self.nps()
            for hp in range(4):
                self.mm(ps[:, hp * 128:(hp + 1) * 128], ARbd[:, hp, c, 0:128], Bbd[:, hp, c, :], True, True,
                        [("ARbd", None), ("Bbd", None)], [pk])
            for hp in range(4):
                self.tt("dve", Np[0][:, hp, :], ps[:, hp * 128:(hp + 1) * 128], mL[:], ALU.mult, [pk, ("c_mL", None)],
                        [("Np0", hp)])
            self.tt("pool", XAs[:], XA[:, :, 1, 0:64], XA[:, :, 1, 64:128], ALU.add, [("XA", None)], [("XAs", None)])
            self.tt("pool", KAs[:], KA[:, :, 1, 0:64], KA[:, :, 1, 64:128], ALU.add, [("KA", None)], [("KAs", None)])

        def doubling(c):
            cur = 0
            for lvl in range(5):
                nx = 1 - cur
                if lvl == 0:
                    Xc, kXc, Rc, kRc = XA[:, :, 0, :], ("XA", None), XA[:, :, 0, :], ("XA", None)
                else:
                    Xc, kXc, Rc, kRc = Xp[cur], ("Xp%d" % cur, None), Rm[cur], ("Rm%d" % cur, None)
                Nc, kNc = Np[cur], ("Np%d" % cur, None)
                Xn, Nn, kXn, kNn = Xp[nx], Np[nx], ("Xp%d" % nx, None), ("Np%d" % nx, None)
                Rn, kRn = Rm[nx], ("Rm%d" % nx, None)
                ps, pk = self.nps()
                for hp in range(4):
                    self.mm(ps[:, hp * 128:(hp + 1) * 128], Nc[:, hp, :], Xc[:, hp, :], True, True, [kNc, kXc], [pk])
                self.cp("act", Xn[:], hp4(ps[:, :]), [pk], [kXn])
                ps, pk = self.nps()
                for hp in range(4):
                    self.mm(ps[:, hp * 128:(hp + 1) * 128], Xc[:, hp, :], Nc[:, hp, :], True, True, [kNc, kXc], [pk])
                self.cp("act", Nn[:], hp4(ps[:, :]), [pk], [kNn])
                self.tt("pool", tmpR[:], Rc, Xn[:], ALU.add, [kRc, kXn], [("tmpR", None)])
                ps, pk = self.nps()
                for hp in range(4):
                    self.mm(ps[:, hp * 128:(hp + 1) * 128], Nn[:, hp, :], Rc[:, hp, :], True, True, [kNn, kRc], [pk])
                self.tt("dve", Rn[:], hp4(ps[:, :]), tmpR[:], ALU.add, [pk, ("tmpR", None)], [kRn])
                cur = nx
                yield
            Rfin[c] = (Rm[cur], ("Rm%d" % cur, None))

        def bk(c):
            pt, pk = self.npt()
            for hp in range(4):
                self.tr(pt[:, hp * 128:(hp + 1) * 128], Bbd[:, hp, c, :], ident[:], [("Bbd", None), ("b_ident", None)], [pk])
            for hp in range(4):
                self.tr(pt[:, 512 + hp * 128:512 + (hp + 1) * 128], Kbd[:, hp, c, :], ident[:],
                        [("Kbd", None), ("b_ident", None)], [pk])
            self.cp("act", BK[:], pt[:, :].rearrange("p (a b c) -> p a b c", a=2, b=4), [pk], [("BKtok", None)])

        def chain(c):
            c64 = slice(c * LR, (c + 1) * LR)
            Rf, kRf = Rfin[c]
            ps, pk = self.nps()
            for hp in range(4):
                o = ps[:, hp * 128:(hp + 1) * 128]
                self.mm(o, ARbd[:, hp, c, 0:128], Sb[:, hp, :], True, False, [("ARbd", None), ("Sb", None)], [pk])
                self.mm(o, KA[:, hp, 0, :], Vbd[:, c, hp, :], False, True, [("KA", hp), ("Vbd", hp)], [pk])
            self.cp("act", Wb[:], hp4(ps[:, :]), [pk], [("Wb", None)])
            ps, pk = self.nps()
            for hp in range(4):
                self.mm(ps[:, hp * 128:(hp + 1) * 128], Rf[:, hp, :], Wb[:, hp, :], True, True, [kRf, ("Wb", None)], [pk])
            self.tt("dve", Ub[:], hp4(ps[:, :]), Wb[:], ALU.add, [pk, ("Wb", None)], [("Ub", None)])
            psy, pky = self.nps()
            for hp in range(4):
                o = psy[0:64, hp * 128:(hp + 1) * 128]
                self.mm(o, rnat[:, hp, c64], Sb[:, hp, :], True, False, [("rnat", hp), ("Sb", None)], [pky])
                self.mm(o, XAs[:, hp, :], Ub[:, hp, :], False, False, [("XAs", None), ("Ub", None)], [pky])
                self.mm(o, KAs[:, hp, :], Vbd[:, c, hp, :], False, True, [("KAs", None), ("Vbd", hp)], [pky])
            pss, pks = self.nps()
            for hp in range(4):
                o = pss[:, hp * 128:(hp + 1) * 128]
                self.mm(o, BK[:, 0, hp, :], Ub[:, hp, :], True, False, [("BKtok", None), ("Ub", None)], [pks])
                self.mm(o, BK[:, 1, hp, :], Vbd[:, c, hp, :], False, True, [("BKtok", None), ("Vbd", hp)], [pks])
            for hp in range(4):
                self.act(tS[:, hp, :], S32[:, hp, :], AF.Copy, [("S32", hp), ("PL", hp)], [("tS", hp)],
                         scale=PL[:, hp, c:c + 1])
                self.stt("dve", S32[:, hp, :], pss[:, hp * 128:(hp + 1) * 128], PL[:, hp, c:c + 1], tS[:, hp, :],
                         ALU.mult, ALU.add, [pks, ("PL", hp), ("tS", hp)], [("S32", hp)])
            self.cp("act", Sb[:], S32[:], [("S32", None)], [("Sb", None)])
            self.cp("act", Y32[0:64, :], psy[0:64, :], [pky], [("Y32", None)])

        def post(c):
            c64 = slice(c * LR, (c + 1) * LR)
            psb, pkb = self.nps()
            for f in range(4):
                self.mm(psb[0:64, 0:8], prod[:, f, c64], t["b_hsel"][:, f, :], f == 0, f == 3, [("prod", f), ("b_hsel", None)], [pkb])
            self.cp("act", bon[0:64, :], psb[0:64, 0:8], [pkb], [("bon", None)])
            ptv, pkv = self.npt()
            for f in range(4):
                self.tr(ptv[0:64, f * 128:(f + 1) * 128], vnat[:, f, c64], ident[:], [("vnat", f), ("b_ident", None)], [pkv])
            psg, pkg = self.psf[5], ("psf5", None)
            self.mm(psg[0:64, :], sxg[:, c64], t["g_up"][:, :], True, True, [("sxg", None), ("g_up", None)], [pkg])
            self.P.add("dve", lambda h: h.reduce_sum(st1[0:64, :], h8(Y32[0:64, :]), AX.X), [("Y32", None)], [("st1", None)])
            self.act(sq32[0:64, :], Y32[0:64, :], AF.Square, [("Y32", None)], [("sq32", None)])
            self.P.add("dve", lambda h: h.reduce_sum(st2[0:64, :], h8(sq32[0:64, :]), AX.X), [("sq32", None)], [("st2", None)])
            yield
            self.ts("dve", st1[0:64, :], st1[0:64, :], 1.0 / 64, None, ALU.mult, None, [("st1", None)], [("st1", None)])
            self.tt("dve", st3[0:64, :], st1[0:64, :], st1[0:64, :], ALU.mult, [("st1", None)], [("st3", None)])
            self.stt("dve", st2[0:64, :], st2[0:64, :], 1.0 / 64, st3[0:64, :], ALU.mult, ALU.subtract,
                     [("st2", None), ("st3", None)], [("st2", None)])
            self.rsqrt_small(st2[0:64, :], st2[0:64, :], 1.0, 64e-5, ("st2", None), ("st2", None))
            yield
            bc = lambda ap: ap.unsqueeze(2).to_broadcast([64, 8, 64])
            self.tt("dve", h8(Y32[0:64, :]), h8(Y32[0:64, :]), bc(st1[0:64, :]), ALU.subtract, [("Y32", None), ("st1", None)],
                    [("Y32", None)])
            yield
            self.tt("dve", h8(Y32[0:64, :]), h8(Y32[0:64, :]), bc(st2[0:64, :]), ALU.mult, [("Y32", None), ("st2", None)],
                    [("Y32", None)])
            self.tt("dve", Y32[0:64, :], Y32[0:64, :], lnb[0:64, 0, :], ALU.mult, [("Y32", None), ("c_ln_bc", None)], [("Y32", None)])
            self.tt("dve", Y32[0:64, :], Y32[0:64, :], lnb[0:64, 1, :], ALU.add, [("Y32", None), ("c_ln_bc", None)], [("Y32", None)])
            yield
            self.tt("dve", h8(sq32[0:64, :]), h8(ptv[0:64, 0:512]), bc(bon[0:64, :]), ALU.mult, [pkv, ("bon", None)], [("sq32", None)])
            self.tt("dve", Y32[0:64, :], Y32[0:64, :], sq32[0:64, :], ALU.add, [("Y32", None), ("sq32", None)], [("Y32", None)])
            self.tt("dve", yr[0:64, :], Y32[0:64, :], psg[0:64, :], ALU.mult, [("Y32", None), pkg], [("yr", None)])
            pt, pk = self.npt()
            for f in range(4):
                self.tr(pt[:, f * 64:(f + 1) * 64], yr[0:64, f * 128:(f + 1) * 128], ident[0:64, 0:64], [("yr", None), ("b_ident", None)], [pk])
            self.cp("act", yT[:, 4:8, c64], pt[:, 0:256].rearrange("p (f t) -> p f t", f=4), [pk], [("yT", ("r", c))])
            yield

        def drain(*gens):
            gens = [g for g in gens if g is not None]
            while gens:
                for g in list(gens):
                    try:
                        next(g)
                    except StopIteration:
                        gens.remove(g)

        prep(0)
        drain(doubling(0))
        bk(0)
        for c in range(NCR):
            chain(c)
            if c + 1 < NCR:
                prep(c + 1)
                drain(doubling(c + 1), post(c))
                bk(c + 1)
            else:
                drain(post(c))

    def phaseB(self):
        t = self.t
        nseq = self.nseq
        TB, NSB = 512, 4
        self.load_consts(["gains", "ident"])
        self.consts_bf(["ident"])
        onesq = self.ab("onesq", [128, 128])
        self.ms("dve", onesq[:], 1.0, [("onesq", None)])
        self.load_w("wq", "wq", D, D)
        self.load_w("wo", "wo", D, D)
        self.af("ht", [128, NSB, D])
        self.af("ssq", [128, NSB])
        self.af("rstd", [128, NSB])
        self.ab("junk", [128, D])
        self.ab("xs0", [128, D])
        self.ab("xs1", [128, D])
        uT = self.ab("uT", [128, KT, TB + H])
        KTx = self.ab("KTx", [128, nseq * 4, 2, MEMT])
        Vx = self.ab("Vx", [128, nseq * 2, D])
        Kmax2 = self.af("Kmax2", [128, nseq * 4])
        qT = self.ab("qT", [128, KT, TB])
        qsq = self.ab("qsq", [128, KT, TB])
        oT = self.ab("oT", [128, KT, TB])
        pTs = [[self.ab("pT%d_%d" % (u, i), [128, TB]) for i in range(2)] for u in range(2)]
        cbs = [self.af("cb%d" % u, [128, TB]) for u in range(2)]
        scs = [[self.af("sc%d_%d" % (u, i), [128, TB]) for i in range(2)] for u in range(2)]
        rinvs = [self.af("rinv%d" % u, [128, TB]) for u in range(2)]
        rinv = rinvs[0]
        mark = self.b_off
        wkv = self.load_w("wkv", "wkv", D, 2 * D)
        ksq = self.ab("ksq", [128, MEMT])
        ht = t["ht"]
        for b in range(nseq):
            mv = self.mem[b * MEMT:(b + 1) * MEMT, :].rearrange("(s p) d -> p s d", p=128)
            self.dma("sp", ht[:, 0:2, :], mv, [], [("ht", None)])
            self.norm_T(3)
            for h in range(4):
                psr, pkr = self.nps()
                for dt in range(2):
                    ps, pk = self.nps()
                    self.proj_fm("wkv", wkv, h * 256 + dt * 128, 128, ps, pk)
                    self.cp("act", KTx[:, b * 4 + h, dt, :], ps[:, 0:MEMT], [pk], [("KTx", (b, h, dt))])
                    self.act(ksq[:], ps[:, 0:MEMT], AF.Square, [pk], [("ksq", None)])
                    self.mm(psr[:, 0:MEMT], onesq[:], ksq[:], dt == 0, dt == 1, [("onesq", None), ("ksq", None)], [pkr])
                self.cp("act", rinv[:, 0:MEMT], psr[:, 0:MEMT], [pkr], [("rinv0", None)])
                self.P.add("dve", lambda h_, o=Kmax2[:, b * 4 + h:b * 4 + h + 1], i=rinv[:, 0:MEMT]: h_.reduce_max(o, i, AX.X),
                           [("rinv0", None)], [("Kmax2", (b, h))])
            for mt in range(2):
                for half in range(2):
                    ps, pk = self.nps()
                    self.proj_tm("wkv", wkv, KT, uT, "uT", mt, D + half * 512, 512, ps, pk, off=H)
                    self.cp("act", Vx[:, b * 2 + mt, half * 512:(half + 1) * 512], ps[:, :], [pk], [("Vx", (b, mt, half))])
        self.P.barrier()
        self.b_off = mark
        self.TT, self.NS = TB, NSB
        for b in range(nseq):
            for ti in range(self.T // TB):
                r0 = b * self.T + ti * TB
                hv = self.h1d[r0:r0 + TB, :].rearrange("(s p) d -> p s d", p=128)
                self.dma("sp", ht[:], hv, [("h1d", None)], [("ht", None)])
                self.norm_T(1)
                for f in range(KT):
                    ps, pk = self.nps()
                    self.proj_fm("wq", t["wq"], f * 128, 128, ps, pk)
                    self.cp("act", qT[:, f, :], ps[:, 0:TB], [pk], [("qT", f)])
                    self.act(qsq[:, f, :], ps[:, 0:TB], AF.Square, [pk], [("qsq", f)])
                def head_stream(h, u):
                    cb_, sc_, pT_, rinv_ = cbs[u], scs[u], pTs[u], rinvs[u]
                    kcb, krv = "cb%d" % u, "rinv%d" % u
                    psr, pkr = self.nps()
                    for dt in range(2):
                        self.mm(psr[:, 0:TB], onesq[:], qsq[:, 2 * h + dt, :], dt == 0, dt == 1,
                                [("onesq", None), ("qsq", 2 * h + dt)], [pkr])
                    self.ts("dve", cb_[:], psr[:, 0:TB], Kmax2[:, b * 4 + h:b * 4 + h + 1], -0.5, ALU.add, ALU.mult,
                            [pkr, ("Kmax2", None)], [(kcb, None)])
                    yield
                    for mt in range(2):
                        ms_ = slice(mt * 128, (mt + 1) * 128)
                        ps, pk = self.nps()
                        for dt in range(2):
                            self.mm(ps[:, 0:TB], KTx[:, b * 4 + h, dt, ms_], qT[:, 2 * h + dt, :], dt == 0, dt == 1,
                                    [("KTx", None), ("qT", 2 * h + dt)], [pk])
                        self.tt("dve", sc_[mt][:], ps[:, 0:TB], cb_[:], ALU.add, [pk, (kcb, None)], [("sc%d_%d" % (u, mt), None)])
                        self.act(pT_[mt][:], sc_[mt][:], AF.Exp, [("sc%d_%d" % (u, mt), None)], [("pT%d_%d" % (u, mt), None)],
                                 scale=1.0 / 16)
                        yield
                    psb, pkb = self.nps()
                    for mt in range(2):
                        self.mm(psb[:, 0:TB], onesq[:], pT_[mt][:], mt == 0, mt == 1,
                                [("onesq", None), ("pT%d_%d" % (u, mt), None)], [pkb])
                    self.cp("act", rinv_[:], psb[:, 0:TB], [pkb], [(krv, None)])
                    yield
                    self.P.add("dve", lambda h_, o=rinv_: h_.reciprocal(o[:], o[:]), [(krv, None)], [(krv, None)])
                    yield
                    for dt in range(2):
                        ps, pk = self.nps()
                        for mt in range(2):
                            self.mm(ps[:, 0:TB], Vx[:, b * 2 + mt, h * 256 + dt * 128:h * 256 + (dt + 1) * 128], pT_[mt][:],
                                    mt == 0, mt == 1, [("Vx", None), ("pT%d_%d" % (u, mt), None)], [pk])
                        self.tt("dve", oT[:, 2 * h + dt, :], ps[:, 0:TB], rinv_[:], ALU.mult, [pk, (krv, None)],
                                [("oT", 2 * h + dt)])
                        yield

                for hpair in range(2):
                    gens = [head_stream(hpair * 2, 0), head_stream(hpair * 2 + 1, 1)]
                    while gens:
                        for g in list(gens):
                            try:
                                next(g)
                            except StopIteration:
                                gens.remove(g)
                for s in range(NSB):
                    for half in range(2):
                        ps, pk = self.nps()
                        self.proj_tm("wo", t["wo"], KT, oT, "oT", s, half * 512, 512, ps, pk)
                        hv2 = ht[:, s, half * 512:(half + 1) * 512]
                        self.tt("dve", hv2, hv2, ps[:, :], ALU.add, [pk, ("ht", s)], [("ht", s)])
                hd = self.h2d[r0:r0 + TB, :].rearrange("(s p) d -> p s d", p=128)
                self.dma("sp", hd, ht[:], [("ht", None)], [("h2d", (b, ti))])
                if self.dbg and "h2" in self.dbg:
                    self.dma("act", self.dbgd["h2"][r0:r0 + TB, :].rearrange("(s p) d -> p s d", p=128), ht[:],
                             [("ht", None)], [("dbg_h2", (b, ti))])

        self.TT, self.NS = TT, NS

    def phaseC(self):
        t = self.t
        TC, NSC = 512, 4
        self.TT, self.NS = TC, NSC
        self.load_consts(["gains", "ident", "nf_bc"])
        self.consts_bf(["ident"])
        self.load_w("wg", "wg", D, DFF)
        self.load_w("wu", "wu", D, DFF)
        self.load_w("wd", "wd", DFF, D)
        ht = self.af("ht", [128, NSC, D])
        self.af("ssq", [128, NSC])
        self.af("rstd", [128, NSC])
        self.ab("junk", [128, D])
        self.ab("xs0", [128, D])
        self.ab("xs1", [128, D])
        self.ab("uT", [128, KT, TC + H])
        hT = self.ab("hT", [128, NFF, TC])
        sgt = [self.af("sgt%d" % i, [128, TC]) for i in range(2)]
        nf = t["c_nf_bc"]
        src = self.h2d if "B" in self.phases else (self.h1d if "A" in self.phases else self.x)
        ntok = self.nseq * self.T
        for r0 in range(0, ntok, TC):
            hv = src[r0:r0 + TC, :].rearrange("(s p) d -> p s d", p=128)
            self.dma("sp", ht[:], hv, [("h2d", None), ("h1d", None)], [("ht", None)])
            self.norm_T(2)
            for f in range(NFF):
                psg, pkg = self.nps()
                self.proj_fm("wg", t["wg"], f * 128, 128, psg, pkg)
                psu, pku = self.nps()
                self.proj_fm("wu", t["wu"], f * 128, 128, psu, pku)
                sg, sk = sgt[f % 2], ("sgt%d" % (f % 2), None)
                self.act(sg[:], psg[:, 0:TC], AF.Silu, [pkg], [sk])
                self.tt("dve", hT[:, f, :], sg[:], psu[:, 0:TC], ALU.mult, [sk, pku], [("hT", f)])
            for s in range(NSC):
                for half in range(2):
                    ps, pk = self.nps()
                    self.proj_tm("wd", t["wd"], NFF, hT, "hT", s, half * 512, 512, ps, pk)
                    hv2 = ht[:, s, half * 512:(half + 1) * 512]
                    self.tt("dve", hv2, hv2, ps[:, :], ALU.add, [pk, ("ht", s)], [("ht", s)])
            for s in range(NSC):
                self.act(t["junk"][:], ht[:, s, :], AF.Square, [("ht", s)], [("junk", None), ("ssq", s)],
                         accum=t["ssq"][:, s:s + 1])
            self.rsqrt_small(t["rstd"][:], t["ssq"][:], 1.0 / D, EPS, ("rstd", None), ("ssq", None))
            for s in range(NSC):
                self.stt("dve", ht[:, s, :], ht[:, s, :], t["rstd"][:, s:s + 1], nf[:], ALU.mult, ALU.mult,
                         [("ht", s), ("rstd", None), ("c_nf_bc", None)], [("ht", s)])
            ov = self.out[r0:r0 + TC, :].rearrange("(s p) d -> p s d", p=128)
            self.dma("sp", ov, ht[:], [("ht", None)], [("out_d", r0)])
        self.TT, self.NS = TT, NS


def host_params(inp):
    f32 = np.float32
    g = np.stack([inp["norm_mix"][0], inp["norm_xattn"][0], inp["norm_ffn"][0], inp["norm_mem"][0]])
    d = {}
    d["gains"] = np.ascontiguousarray(g.reshape(4, KT, 128).transpose(2, 0, 1))
    d["convw"] = np.ascontiguousarray(inp["mlstm_conv"][0].T.reshape(8, 128, 4).transpose(1, 0, 2))
    d["gb"] = np.ascontiguousarray(np.stack([inp["mlstm_i_bias"][0], inp["mlstm_f_bias"][0]], axis=1))
    d["nw_bc"] = np.ascontiguousarray(np.broadcast_to(inp["mlstm_norm"][0][None, :], (128, 512)))
    mu = inp["rwkv_mu"][0]
    mu15 = np.zeros((128, 15), f32)
    for j, (off, M) in enumerate(RW):
        mu15[:M, j] = mu[off - OFF_R:off - OFF_R + M]
    d["mu15"] = mu15
    pp = np.zeros((128, 6, 4), f32)
    for i, k in enumerate(["rwkv_w0", "rwkv_a0", "rwkv_k_k", "rwkv_k_a", "rwkv_r_k"]):
        pp[:, i, :] = inp[k][0].reshape(4, 128).T
    d["pp"] = pp
    d["ln_bc"] = np.ascontiguousarray(np.broadcast_to(
        np.stack([inp["rwkv_ln_w"][0], inp["rwkv_ln_b"][0]])[None], (128, 2, 512)))
    d["nf_bc"] = np.ascontiguousarray(np.broadcast_to(inp["norm_final"][None, :], (128, D)))
    i128 = np.arange(128)
    d["ident"] = np.eye(128, dtype=f32)
    d["maskU"] = (i128[:, None] <= i128[None, :]).astype(f32)
    l64 = i128 % 64
    strict = (l64[:, None] < l64[None, :]).astype(f32)
    incl = (l64[:, None] <= l64[None, :]).astype(f32)
    d["mXA"] = np.ascontiguousarray(np.stack([strict, incl], axis=1))
    d["mL"] = (l64[:, None] > l64[None, :]).astype(f32)
    sel = np.zeros((4, 8, 128), f32)
    for h in range(4):
        sel[h, h, :] = 1.0
        sel[h, 4 + h, :] = 128.0 ** -0.5
    d["sel"] = sel
    tt_ = np.arange(TT)
    d["mask4"] = np.ascontiguousarray(np.broadcast_to((tt_ % LM != 0).astype(f32)[None], (4, TT)))
    d["mask64"] = np.ascontiguousarray(np.broadcast_to((tt_ % LR != 0).astype(f32)[None], (128, TT)))
    d["blockones"] = ((i128[:, None] // 64) == (i128[None, :] // 64)).astype(f32)
    hsel = np.zeros((128, 4, 8), f32)
    for f in range(4):
        hsel[:64, f, 2 * f] = 1.0
        hsel[64:, f, 2 * f + 1] = 1.0
    d["hsel"] = hsel
    w = {"w_in": "w_in", "w_mix": "w_mix_out", "wq": "xattn_wq", "wkv": "xattn_wkv", "wo": "xattn_wo",
         "wg": "ffn_w_gate", "wu": "ffn_w_up", "wd": "ffn_w_down", "w_up": "rwkv_w_up", "a_up": "rwkv_a_up",
         "g_up": "rwkv_g_up"}
    for k, src in w.items():
        d[k] = np.ascontiguousarray(inp[src][0])
    for k in d:
        d[k] = np.ascontiguousarray(d[k], dtype=f32)
    return d


def kernel(**inp):
    ncores = 8
    B, T, _ = inp["x"].shape
    nseq = B // ncores
    kb = K(nseq, T)
    nc = kb.build()
    shared = host_params(inp)
    in_maps = []
    for c in range(ncores):
        m = dict(shared)
        m["x"] = np.ascontiguousarray(inp["x"][c * nseq:(c + 1) * nseq].reshape(nseq * T, D), dtype=np.float32)
        m["mem"] = np.ascontiguousarray(inp["mem"][c * nseq:(c + 1) * nseq].reshape(nseq * MEMT, D), dtype=np.float32)
        in_maps.append(m)
    res = run_bass_kernel_spmd(nc, in_maps, core_ids=list(range(ncores)))
    out = np.concatenate([np.asarray(r["out"]).reshape(nseq, T, D) for r in res.results], axis=0)
    return out.astype(np.float32)
```

```python
import contextlib
import numpy as np
import concourse.bass as bass
import concourse.mybir as mybir
from concourse.bass_utils import run_bass_kernel_spmd

F32 = mybir.dt.float32
BF16 = mybir.dt.bfloat16
ALU = mybir.AluOpType
AF = mybir.ActivationFunctionType
AX = mybir.AxisListType

class Prog:
    ENG = ("pe", "act", "dve", "pool", "sp")

    def __init__(self):
        self.ops = {e: [] for e in self.ENG}
        self.known = {e: {} for e in self.ENG}
        self.reg = {}
        self.used = set()
        self.dma_rr = {}
        self.dma_cnt = {}
        self.ndma_sems = 8

    def _subs(self, key):
        name, sub = key
        d = self.reg.setdefault(name, {})
        if "*" not in d:
            d["*"] = [None, {}]
        if sub is None:
            return [d[k] for k in d]
        if sub not in d:
            star = d["*"]
            d[sub] = [star[0], dict(star[1])]
        return [d[sub]]

    def _deps(self, reads, writes):
        deps = {}

        def add(evt):
            if evt is None:
                return
            s, i = evt
            if deps.get(s, 0) < i:
                deps[s] = i

        for k in reads:
            for st in self._subs(k):
                add(st[0])
        for k in writes:
            for st in self._subs(k):
                add(st[0])
                for s, i in st[1].items():
                    add((s, i))
        return deps

    def _register(self, evt, reads, writes):
        s, i = evt
        for k in reads:
            for st in self._subs(k):
                if st[1].get(s, 0) < i:
                    st[1][s] = i
        for k in writes:
            for st in self._subs(k):
                st[0] = evt
                st[1] = {}

    def add(self, eng, fn, reads=(), writes=(), dma=False, dma_sem=None):
        reads = list(reads)
        writes = list(writes)
        deps = self._deps(reads, writes)
        kn = self.known[eng]
        own = "E_" + eng
        waits = []
        if dma:
            rr = self.dma_rr.get(eng, 0)
            self.dma_rr[eng] = rr + 1
            sem = dma_sem or ("D_%s_%d" % (eng, rr % self.ndma_sems))
            n = self.dma_cnt.get(sem, 0)
            if n > 0 and dma_sem is None:
                deps[sem] = max(deps.get(sem, 0), n)
            self.dma_cnt[sem] = n + 1
            evt = (sem, n + 1)
        else:
            evt = (own, len(self.ops[eng]) + 1)
        for s, i in deps.items():
            if eng == "pe" and s == own:
                continue
            if kn.get(s, 0) >= i:
                continue
            kn[s] = i
            waits.append((s, i))
            self.used.add((s, i))
        if dma:
            self.used.add(evt)
        self.ops[eng].append(dict(fn=fn, waits=waits, evt=evt, dma=dma))
        self._register(evt, reads, writes)
        return evt

    def wait_all(self, eng, keys):
        self.add(eng, None, reads=keys, writes=())

    def emit(self, nc):
        semnames = set()
        for e in self.ENG:
            semnames.add("E_" + e)
        for s in self.dma_cnt:
            semnames.add(s)
        ranks = {}
        for e in self.ENG:
            own = "E_" + e
            idxs = sorted(i for (s, i) in self.used if s == own)
            ranks[own] = {i: r + 1 for r, i in enumerate(idxs)}
        with contextlib.ExitStack() as st:
            sems = {n: st.enter_context(nc.semaphore(n)) for n in sorted(semnames)}
            block = st.enter_context(nc.Block())

            def val(s, i):
                return ranks[s][i] if s.startswith("E_") else 16 * i

            def run(e, h):
                for op in self.ops[e]:
                    waits = [(sems[s], val(s, i)) for (s, i) in op["waits"]]
                    if op["fn"] is None:
                        for sh, v in waits:
                            h.wait_ge(sh, v)
                        continue
                    for sh, v in waits[1:]:
                        h.wait_ge(sh, v)
                    ins = op["fn"](h)
                    if waits:
                        ins._wait_ge(waits[0][0], waits[0][1])
                    evt = op["evt"]
                    if op["dma"]:
                        ins.then_inc(sems[evt[0]], 16)
                    elif evt in self.used:
                        ins.then_inc(sems[evt[0]], 1)

            @block.tensor
            def _(h):
                run("pe", h)

            @block.scalar
            def _(h):
                run("act", h)

            @block.vector
            def _(h):
                run("dve", h)

            @block.gpsimd
            def _(h):
                run("pool", h)

            @block.sync
            def _(h):
                run("sp", h)

    def count(self):
        return {e: len(v) for e, v in self.ops.items()}

    def barrier(self):
        last = {}
        for e in self.ENG:
            n = 0
            for i, op in enumerate(self.ops[e]):
                if op["fn"] is not None and not op["dma"]:
                    n = i + 1
            if n:
                last["E_" + e] = n
        for s, n in self.dma_cnt.items():
            last[s] = n
        for e in self.ENG:
            waits = []
            for s, i in last.items():
                if self.known[e].get(s, 0) >= i:
                    continue
                self.known[e][s] = i
                waits.append((s, i))
                self.used.add((s, i))
            self.ops[e].append(dict(fn=None, waits=waits, evt=("E_" + e, len(self.ops[e]) + 1), dma=False))
        self.reg = {}

D = 1024
KT = 8
TT = 256
NS = 2
DIN = 3848
DFF = 2816
NFF = 22
H = 16
MEMT = 256
EPS = 1e-6
OFF_V, OFF_O, OFF_IG, OFF_FG, OFF_R = 1024, 1536, 2048, 2052, 2056
RW = [(OFF_R + 128 * j, 128) for j in range(12)] + [(OFF_R + 1536, 64), (OFF_R + 1600, 64), (OFF_R + 1664, 128)]
DEC = 0.6065306597126334
LM = 128
LR = 64
NCM = TT // LM
NCR = TT // LR
WEIGHTS = {"w_in": (D, DIN), "w_up": (64, 512), "a_up": (64, 512), "g_up": (128, 512), "w_mix": (D, D),
           "wq": (D, D), "wkv": (D, 2 * D), "wo": (D, D), "wg": (D, DFF), "wu": (D, DFF), "wd": (DFF, D)}
SMALL = {"gains": (128, 4, KT), "convw": (128, 8, 4), "gb": (4, 2), "nw_bc": (128, 512), "mu15": (128, 15),
         "pp": (128, 6, 4), "ln_bc": (128, 2, 512), "nf_bc": (128, D), "ident": (128, 128), "maskU": (128, 128),
         "mXA": (128, 2, 128), "mL": (128, 128), "sel": (4, 8, 128), "mask4": (4, TT), "mask64": (128, TT),
         "blockones": (128, 128), "hsel": (128, 4, 8)}


class K:
    def __init__(self, nseq, T, dbg=None, phases="ABC"):
        self.nseq, self.T, self.dbg, self.phases = nseq, T, dbg, phases
        self.TT, self.NS = TT, NS
        self.nc = bass.Bass("TRN2", target_bir_lowering=False)
        self.P = Prog()
        self.st = contextlib.ExitStack()
        self.t = {}

    def sb(self, name, shape, dt=F32):
        return self.st.enter_context(self.nc.sbuf_tensor(name, list(shape), dt))

    def ps(self, name, shape, dt=F32):
        return self.st.enter_context(self.nc.psum_tensor(name, list(shape), dt))

    @staticmethod
    def _shape(v, shape):
        if len(shape) == 2:
            return v
        if len(shape) == 3:
            return v.rearrange("p (a b) -> p a b", a=shape[1])
        if len(shape) == 4:
            return v.rearrange("p (a b c) -> p a b c", a=shape[1], b=shape[2])
        raise ValueError(shape)

    def ab(self, name, shape):
        n = int(np.prod(shape[1:]))
        off = self.b_off
        self.b_off += (n + 7) // 8 * 8
        assert self.b_off <= self.NB, ("arena overflow", name, self.b_off)
        v = self._shape(self.arena_b[:, off:off + n], shape)
        self.t[name] = v
        return v

    def af(self, name, shape):
        n = int(np.prod(shape[1:]))
        off = self.b_off
        self.b_off += (2 * n + 7) // 8 * 8
        assert self.b_off <= self.NB, ("arena overflow", name, self.b_off)
        v = self._shape(self.arena_b[:, off:off + 2 * n].bitcast(F32), shape)
        self.t[name] = v
        return v

    def mm(self, out, lhsT, rhs, start, stop, r, w):
        self.P.add("pe", lambda h: h.matmul(out, lhsT, rhs, start=start, stop=stop), r, w)

    def tr(self, out, in_, ident, r, w):
        self.P.add("pe", lambda h: h.transpose(out, in_, ident), r, w)

    def act(self, out, in_, func, r, w, bias=0.0, scale=1.0, accum=None):
        if accum is None:
            self.P.add("act", lambda h: h.activation(out, in_, func, bias=bias, scale=scale), r, w)
        else:
            self.P.add("act", lambda h: h.activation(out, in_, func, bias=bias, scale=scale,
                                                     accum_out=accum), r, w)

    def ts(self, eng, out, in0, s1, s2, op0, op1, r, w):
        if op1 is None:
            self.P.add(eng, lambda h: h.tensor_scalar(out, in0, s1, None, op0), r, w)
        else:
            self.P.add(eng, lambda h: h.tensor_scalar(out, in0, s1, s2, op0, op1), r, w)

    def tt(self, eng, out, in0, in1, op, r, w):
        self.P.add(eng, lambda h: h.tensor_tensor(out, in0, in1, op), r, w)

    def stt(self, eng, out, in0, scalar, in1, op0, op1, r, w):
        self.P.add(eng, lambda h: h.scalar_tensor_tensor(out, in0, scalar, in1, op0, op1), r, w)

    def cp(self, eng, out, in_, r, w):
        if eng == "act":
            self.P.add("act", lambda h: h.copy(out, in_), r, w)
        else:
            self.P.add(eng, lambda h: h.tensor_copy(out, in_), r, w)

    def ms(self, eng, out, val, w):
        self.P.add(eng, lambda h: h.memset(out, val), [], w)

    def dma(self, q, out, in_, r, w):
        self.P.add(q, lambda h: h.dma_start(out=out, in_=in_), r, w, dma=True)

    def rsqrt_small(self, out, in_, mul, add, kout, kin):
        self.ts("dve", out, in_, float(mul), float(add), ALU.mult, ALU.add, [kin], [kout])
        self.act(out, out, AF.Ln, [kout], [kout])
        self.act(out, out, AF.Exp, [kout], [kout], scale=-0.5)

    def nps(self):
        i = self.ps_rr % 5
        self.ps_rr += 1
        return self.psf[i], ("psf%d" % i, None)

    def npt(self):
        i = self.pt_rr % len(self.pst)
        self.pt_rr += 1
        return self.pst[i], ("pst%d" % i, None)

    def dq(self):
        self.dq_rr += 1
        return "sp" if self.dq_rr % 2 else "act"

    def build(self):
        nseq, T = self.nseq, self.T
        NTOK = nseq * T
        nc = self.nc
        di = lambda n, s, dt=F32: nc.dram_tensor(n, list(s), dt, kind="ExternalInput").ap()
        self.x = di("x", [NTOK, D])
        self.mem = di("mem", [nseq * MEMT, D])
        self.wsrc = {k: di(k, v) for k, v in WEIGHTS.items()}
        self.ssrc = {k: di(k, v) for k, v in SMALL.items()}
        self.out = nc.dram_tensor("out", [NTOK, D], F32, kind="ExternalOutput").ap()
        self.wbf = {k: nc.dram_tensor("bf_" + k, list(v), BF16, kind="Internal").ap() for k, v in WEIGHTS.items()}
        self.h1d = nc.dram_tensor("h1d", [NTOK, D], F32, kind="Internal").ap()
        self.h2d = nc.dram_tensor("h2d", [NTOK, D], F32, kind="Internal").ap()
        if self.dbg:
            self.dbgd = {k: nc.dram_tensor("dbg_" + k, list(s), F32, kind="ExternalOutput").ap()
                         for k, s in self.dbg.items()}

        self.NB = 104000
        self.arena_b = self.sb("arena_b", [128, self.NB], BF16)
        self.psf = [self.ps("psf%d" % i, [128, 512]) for i in range(6)]
        self.pst = [self.ps("pst%d" % i, [128, 1024], BF16) for i in range(2)]
        self.ps_rr = self.pt_rr = self.dq_rr = 0
        self.b_off = self.f_off = 0

        wk = []
        for k, (rows, cols) in WEIGHTS.items():
            for r0 in range(0, rows, 128):
                r1 = min(rows, r0 + 128)
                self.P.add("pool", lambda h, o=self.wbf[k][r0:r1, :], i=self.wsrc[k][r0:r1, :]: h.dma_start(out=o, in_=i),
                           [], [("wbf_" + k, r0)], dma=True, dma_sem="DW_" + k)
            wk.append(("wbf_" + k, None))
        scr = self.af("scr", [128, 4])
        self.P.add("pool", lambda h: h.memset(scr[:, 1:2], 0.0), [], [("scr", 1)])
        self.P.add("dve", lambda h: h.memset(scr[:, 0:1], 0.0), [("scr", 1)], [("scr", 0)])

        if "A" in self.phases:
            self.phaseA()
        self.P.barrier()
        self.b_off = 0
        if "B" in self.phases:
            self.phaseB()
        self.P.barrier()
        self.b_off = 0
        if "C" in self.phases:
            self.phaseC()
        self.P.wait_all("sp", [("out_d", None)] + [("dbg_" + k, None) for k in (self.dbg or {})])
        self.P.emit(nc)
        self.st.close()
        return nc

    def load_consts(self, names):
        for k in names:
            shp = SMALL[k]
            v = self.af("c_" + k, [128] + list(shp[1:]))
            self.dma(self.dq(), v[0:shp[0]], self.ssrc[k], [], [("c_" + k, None)])

    def consts_bf(self, names):
        for k in names:
            shp = SMALL[k]
            v = self.ab("b_" + k, [128] + list(shp[1:]))
            self.cp("act", v[0:shp[0]], self.t["c_" + k][0:shp[0]], [("c_" + k, None)], [("b_" + k, None)])

    def load_w(self, key, name, rows, cols):
        if rows <= 128:
            v = self.ab(name, [128, cols])
            self.dma(self.dq(), v[0:rows, :], self.wbf[key], [("wbf_" + key, None)], [(name, None)])
            return v
        kt = rows // 128
        v = self.ab(name, [128, kt, cols])
        src = self.wbf[key].rearrange("(k p) n -> p k n", p=128)
        step = max(1, kt // 4)
        for k0 in range(0, kt, step):
            k1 = min(kt, k0 + step)
            self.dma(self.dq(), v[:, k0:k1, :], src[:, k0:k1, :], [("wbf_" + key, None)], [(name, (k0,))])
        return v

    def dbg_dump(self, key, dst_ap, src_ap, rkeys):
        if self.dbg and key in self.dbg:
            self.dma("sp", dst_ap, src_ap, rkeys, [("dbg_" + key, None)])

    def norm_T(self, gi):
        t = self.t
        NS_ = self.NS
        ht, ssq, rstd, junk, uT, gains, ident = t["ht"], t["ssq"], t["rstd"], t["junk"], t["uT"], t["c_gains"], t["b_ident"]
        for s in range(NS_):
            self.act(junk[:], ht[:, s, :], AF.Square, [("ht", s)], [("junk", None), ("ssq", s)],
                     accum=ssq[:, s:s + 1])
        self.rsqrt_small(rstd[:, 0:NS_], ssq[:, 0:NS_], 1.0 / D, EPS, ("rstd", None), ("ssq", None))
        for s in range(NS_):
            xs = t["xs%d" % (s % 2)]
            xk = ("xs%d" % (s % 2), None)
            self.act(xs[:], ht[:, s, :], AF.Copy, [("ht", s), ("rstd", None)], [xk], scale=rstd[:, s:s + 1])
            for half in range(2):
                pt, pk = self.npt()
                for j in range(4):
                    k = half * 4 + j
                    self.tr(pt[:, j * 128:(j + 1) * 128], xs[:, k * 128:(k + 1) * 128], ident[:],
                            [xk, ("b_ident", None)], [pk])
                for j in range(4):
                    k = half * 4 + j
                    o = uT[:, k, H + s * 128:H + (s + 1) * 128]
                    if False:
                        self.act(o, pt[:, j * 128:(j + 1) * 128], AF.Copy, [pk, ("c_gains", None)], [("uT", (k, s))],
                                 scale=gains[:, gi, k:k + 1])
                    else:
                        self.ts("dve", o, pt[:, j * 128:(j + 1) * 128], gains[:, gi, k:k + 1], None, ALU.mult, None,
                                [pk, ("c_gains", None)], [("uT", (k, s))])

    def proj_fm(self, wname, w, c0, M, ps, pk, h0=H):
        TT_ = self.TT
        for k in range(KT):
            self.mm(ps[0:M, 0:TT_ + H - h0], w[:, k, c0:c0 + M], self.t["uT"][:, k, h0:TT_ + H], k == 0, k == KT - 1,
                    [(wname, None), ("uT", None)], [pk])

    def proj_tm(self, wname, w, nk, lhs, lname, s, c0, N, ps, pk, off=0):
        for k in range(nk):
            self.mm(ps[:, 0:N], lhs[:, k, off + s * 128:off + (s + 1) * 128], w[:, k, c0:c0 + N], k == 0, k == nk - 1,
                    [(wname, None), (lname, None)], [pk])

    def phaseA(self):
        t = self.t
        self.load_consts(["gains", "convw", "gb", "nw_bc", "mu15", "pp", "ln_bc", "ident", "maskU", "mXA", "mL", "sel",
                          "mask4", "mask64", "blockones", "hsel"])
        self.consts_bf(["ident", "blockones", "hsel"])
        id4 = self.ab("id4", [128, 4, 128])
        for i in range(4):
            self.cp("act", id4[:, i, :], t["c_ident"][:], [("c_ident", None)], [("id4", i)])
        self.load_w("w_in", "w_in", D, DIN)
        self.load_w("w_mix", "w_mix", D, D)
        self.load_w("w_up", "w_up", 64, 512)
        self.load_w("a_up", "a_up", 64, 512)
        self.load_w("g_up", "g_up", 128, 512)
        self.af("ht", [128, NS, D])
        self.af("ssq", [128, NS])
        self.af("rstd", [128, NS])
        self.ab("junk", [128, D])
        self.ab("xs0", [128, D])
        self.ab("xs1", [128, D])
        self.ab("uT", [128, KT, TT + H])
        self.ab("yT", [128, KT, TT])
        C32 = self.af("C32", [128, 4, 129])
        Cb = self.ab("Cb", [128, 4, 129])
        S32 = self.af("S32", [128, 4, 128])
        Sb = self.ab("Sb", [128, 4, 128])
        nfb = self.af("nfb", [128, 1])
        omka = self.af("omka", [128, 4])
        v_aug = self.ab("v_aug", [128, NCM, 4, 129])
        Bbd = self.ab("Bbd", [128, 4, NCR, 128])
        Kbd = self.ab("Kbd", [128, 4, NCR, 128])
        ARbd = self.ab("ARbd", [128, 4, NCR, 256])
        Vbdt = self.ab("Vbdt", [128, 4, NCR, 128])
        self.ts("dve", nfb[0:4, :], t["c_gb"][0:4, 1:2], -1.0, None, ALU.mult, None, [("c_gb", None)], [("nfb", None)])
        self.ts("dve", omka[:], t["c_pp"][:, 3, :], -1.0, 1.0, ALU.mult, ALU.add, [("c_pp", None)], [("omka", None)])
        self.ts("dve", t["c_nw_bc"][:], t["c_nw_bc"][:], 0.5, None, ALU.mult, None, [("c_nw_bc", None)], [("c_nw_bc", None)])
        self.ms("dve", v_aug[:], 1.0, [("v_aug", None)])
        for nm in ("Bbd", "Kbd", "ARbd", "Vbdt"):
            self.ms("dve", t[nm][:], 0.0, [(nm, None)])
        self.markA = self.b_off
        for b in range(self.nseq):
            self.ms("dve", C32[:], 0.0, [("C32", None)])
            self.ms("dve", Cb[:], 0.0, [("Cb", None)])
            self.ms("dve", S32[:], 0.0, [("S32", None)])
            self.ms("dve", Sb[:], 0.0, [("Sb", None)])
            self.ms("dve", t["uT"][:, :, 0:H], 0.0, [("uT", "h")])
            for ti in range(self.T // TT):
                self.tileA(b, ti)

    def tileA(self, b, ti):
        t = self.t
        r0 = b * self.T + ti * TT
        self.b_off = self.markA
        xv = self.x[r0:r0 + TT, :].rearrange("(s p) d -> p s d", p=128)
        self.dma("sp", t["ht"][:], xv, [], [("ht", None)])
        if ti > 0:
            self.cp("act", t["uT"][:, :, 0:H], t["uT"][:, :, TT:TT + H], [("uT", None)], [("uT", "h")])
        self.norm_T(0)
        self.P.barrier()
        self.mlstm()
        self.P.barrier()
        self.b_off = self.markA
        self.rwkv()
        self.b_off = self.markA
        if self.dbg and "yT" in self.dbg:
            for k in range(KT):
                yd = self.af("yd%d" % k, [128, TT])
                self.cp("act", yd[:], t["yT"][:, k, :], [("yT", None)], [("yd%d" % k, None)])
                self.dma("sp", self.dbgd["yT"][k * 128:(k + 1) * 128, r0:r0 + TT], yd[:], [("yd%d" % k, None)],
                         [("dbg_yT", (k, r0))])
        for s in range(NS):
            for half in range(2):
                ps, pk = self.nps()
                self.proj_tm("w_mix", t["w_mix"], KT, t["yT"], "yT", s, half * 512, 512, ps, pk)
                hv = t["ht"][:, s, half * 512:(half + 1) * 512]
                self.tt("dve", hv, hv, ps[:, :], ALU.add, [pk, ("ht", s)], [("ht", s)])
        hd = self.h1d[r0:r0 + TT, :].rearrange("(s p) d -> p s d", p=128)
        self.dma("sp", hd, t["ht"][:], [("ht", None)], [("h1d", (b, ti))])

    def mlstm(self):
        t = self.t
        w_in, uT, ident = t["w_in"], t["uT"], t["b_ident"]
        convw, gb, nfb = t["c_convw"], t["c_gb"], t["nfb"]
        C32, Cb, v_aug, yT = t["C32"], t["Cb"], t["v_aug"], t["yT"]
        qk = self.ab("qk", [128, 8, TT])
        qkt = self.ab("qkt", [128, 8, TT])
        ktok = self.ab("ktok", [128, 4, NCM, 128])
        gate = self.ab("gate", [128, NS, 512])
        PT = self.ab("PT", [128, 4, NCM, 128])
        ymt = [self.ab("ym%d" % i, [128, 512]) for i in range(2)]
        zt = [self.af("zt%d" % i, [128, TT + H]) for i in range(2)]
        acc = [self.af("acc%d" % i, [128, TT]) for i in range(2)]
        so = self.af("so", [128, 512])
        ig = self.af("ig", [128, TT])
        e1 = self.af("e1", [128, TT])
        bneg = self.af("bneg", [128, TT])
        eb = self.af("eb", [128, TT])
        ek = self.af("ek", [128, TT])
        eg = self.af("eg", [128, 4, NCM])
        Cegs = [self.af("Ceg%d" % i, [128, 2, 129]) for i in range(2)]
        dds = [self.af("dd%d" % i, [128, 2]) for i in range(2)]
        ss2s = [self.af("ss2%d" % i, [128, 2]) for i in range(2)]
        hns = [self.af("hn%d" % i, [128, 2, 128]) for i in range(2)]
        psi, pki = self.nps()
        self.proj_fm("w_in", w_in, OFF_IG, 4, psi, pki)
        psg, pkg = self.nps()
        self.proj_fm("w_in", w_in, OFF_FG, 4, psg, pkg)
        self.act(ig[0:4, :], psi[0:4, 0:TT], AF.Identity, [pki, ("c_gb", None)], [("ig", None)], bias=gb[0:4, 0:1])
        self.act(e1[0:4, :], psg[0:4, 0:TT], AF.Exp, [pkg, ("nfb", None)], [("e1", None)], bias=nfb[0:4, 0:1], scale=-1.0)
        self.act(e1[0:4, :], e1[0:4, :], AF.Ln, [("e1", None)], [("e1", None)], bias=1.0)
        m4 = t["c_mask4"]
        self.P.add("dve", lambda h: h.tensor_tensor_scan(bneg[0:4, :], m4[0:4, :], e1[0:4, :], 0.0, ALU.mult, ALU.add),
                   [("e1", None), ("c_mask4", None)], [("bneg", None)])
        self.act(eb[0:4, :], bneg[0:4, :], AF.Exp, [("bneg", None)], [("eb", None)], scale=-1.0)
        self.tt("dve", ek[0:4, :], ig[0:4, :], bneg[0:4, :], ALU.add, [("ig", None), ("bneg", None)], [("ek", None)])
        self.act(ek[0:4, :], ek[0:4, :], AF.Exp, [("ek", None)], [("ek", None)])
        for fp in range(4):
            grp = []
            for i in range(2):
                f = fp * 2 + i
                ps, pk = self.nps()
                self.proj_fm("w_in", w_in, f * 128, 128, ps, pk, h0=0)
                z, zk = zt[i], ("zt%d" % i, None)
                a, ak = acc[i], ("acc%d" % i, None)
                self.act(z[:, 0:TT + H], ps[:, 0:TT + H], AF.Copy, [pk], [zk])
                grp.append((f, z, zk, a, ak))
            for (f, z, zk, a, ak) in grp:
                self.ts("dve", a[:], z[:, H - 3:H - 3 + TT], convw[:, f, 0:1], None, ALU.mult, None, [zk, ("c_convw", None)], [ak])
            for j in range(1, 4):
                for (f, z, zk, a, ak) in grp:
                    self.stt("dve", a[:], z[:, H - 3 + j:H - 3 + j + TT], convw[:, f, j:j + 1], a[:], ALU.mult, ALU.add,
                             [zk, ak, ("c_convw", None)], [ak])
            for (f, z, zk, a, ak) in grp:
                self.act(qk[:, f, :], a[:], AF.Silu, [ak], [("qk", f)])
        for s in range(NS):
            ps, pk = self.nps()
            self.proj_tm("w_in", w_in, KT, uT, "uT", s, OFF_V, 512, ps, pk, off=H)
            self.act(v_aug[:, s, :, 0:128], ps[:, :].rearrange("p (h d) -> p h d", h=4), AF.Copy, [pk], [("v_aug", s)])
            ps, pk = self.nps()
            self.proj_tm("w_in", w_in, KT, uT, "uT", s, OFF_O, 512, ps, pk, off=H)
            self.act(so[:], ps[:, :], AF.Tanh, [pk], [("so", None)], scale=0.5)
            self.stt("dve", gate[:, s, :], so[:], 1.0, t["c_nw_bc"][:], ALU.add, ALU.mult, [("so", None), ("c_nw_bc", None)],
                     [("gate", s)])
        sel = t["c_sel"]
        for h in range(4):
            ps, pk = self.nps()
            self.mm(ps[:, 0:TT], sel[0:4, h, :], eb[0:4, :], True, True, [("c_sel", None), ("eb", None)], [pk])
            self.tt("dve", qkt[:, h, :], qk[:, h, :], ps[:, 0:TT], ALU.mult, [("qk", h), pk], [("qkt", h)])
            self.cp("act", eg[:, h, :], ps[:, 0:TT].rearrange("p (c t) -> p c t", t=LM)[:, :, LM - 1], [pk], [("eg", h)])
            ps2, pk2 = self.nps()
            self.mm(ps2[:, 0:TT], sel[0:4, 4 + h, :], ek[0:4, :], True, True, [("c_sel", None), ("ek", None)], [pk2])
            self.tt("dve", qkt[:, 4 + h, :], qk[:, 4 + h, :], ps2[:, 0:TT], ALU.mult, [("qk", 4 + h), pk2], [("qkt", 4 + h)])
        for h in range(4):
            pt, pk = self.npt()
            for c in range(NCM):
                self.tr(pt[:, c * 128:(c + 1) * 128], qkt[:, 4 + h, c * 128:(c + 1) * 128], ident[:],
                        [("qkt", 4 + h), ("b_ident", None)], [pk])
            self.cp("act", ktok[:, h, :, :], pt[:, 0:NCM * 128].rearrange("p (c e) -> p c e", c=NCM), [pk], [("ktok", h)])
            ps, pk = self.nps()
            for c in range(NCM):
                cs = slice(c * 128, (c + 1) * 128)
                self.mm(ps[:, cs], qkt[:, 4 + h, cs], qkt[:, h, cs], True, True, [("qkt", 4 + h), ("qkt", h)], [pk])
            for c in range(NCM):
                self.tt("dve", PT[:, h, c, :], ps[:, c * 128:(c + 1) * 128], t["c_maskU"][:], ALU.mult,
                        [pk, ("c_maskU", None)], [("PT", (h, c))])
        def hp_stream(c, hp, ym, ymk):
            cs = slice(c * 128, (c + 1) * 128)
            dd, ss2, hn, Ceg = dds[hp], ss2s[hp], hns[hp], Cegs[hp]
            kd, ks, kh, kc = "dd%d" % hp, "ss2%d" % hp, "hn%d" % hp, "Ceg%d" % hp
            psn, pkn = self.nps()
            pss, pks = self.nps()
            for j in range(2):
                h = hp * 2 + j
                js = slice(j * 129, (j + 1) * 129)
                self.mm(psn[:, js], PT[:, h, c, :], v_aug[:, c, h, :], True, False,
                        [("PT", (h, c)), ("v_aug", c)], [pkn])
                self.mm(psn[:, js], qkt[:, h, cs], Cb[:, h, :], False, True, [("qkt", h), ("Cb", h)], [pkn])
                self.mm(pss[:, js], ktok[:, h, c, :], v_aug[:, c, h, :], True, True,
                        [("ktok", h), ("v_aug", c)], [pks])
            yield
            nv = psn[:, 0:258].rearrange("p (j d) -> p j d", j=2)
            self.act(dd[:], nv[:, :, 128], AF.Abs, [pkn], [(kd, None)])
            for j in range(2):
                h = hp * 2 + j
                self.act(Ceg[:, j, :], C32[:, h, :], AF.Copy, [("C32", h), ("eg", h)], [(kc, j)],
                         scale=eg[:, h, c:c + 1])
            yield
            self.ts("dve", dd[:], dd[:], 1.0, None, ALU.max, None, [(kd, None)], [(kd, None)])
            for j in range(2):
                h = hp * 2 + j
                js = slice(j * 129, (j + 1) * 129)
                self.stt("dve", C32[:, h, :], pss[:, js], eg[:, h, c:c + 1], Ceg[:, j, :], ALU.mult, ALU.add,
                         [pks, ("eg", h), (kc, j)], [("C32", h)])
            yield
            self.P.add("dve", lambda h_, dd=dd: h_.reciprocal(dd[:], dd[:]), [(kd, None)], [(kd, None)])
            for j in range(2):
                h = hp * 2 + j
                self.cp("act", Cb[:, h, :], C32[:, h, :], [("C32", h)], [("Cb", h)])
            yield
            for j in range(2):
                self.ts("dve", hn[:, j, :], nv[:, j, 0:128], dd[:, j:j + 1], None, ALU.mult, None,
                        [pkn, (kd, None)], [(kh, j)])
            yield
            for j in range(2):
                jk = ("junk", (hp, j))
                self.act(t["junk"][:, (hp * 2 + j) * 128:(hp * 2 + j + 1) * 128], hn[:, j, :], AF.Square, [(kh, j)],
                         [jk, (ks, j)], accum=ss2[:, j:j + 1])
            yield
            self.ts("dve", ss2[:], ss2[:], 1.0 / 128, EPS, ALU.mult, ALU.add, [(ks, None)], [(ks, None)])
            yield
            self.act(ss2[:], ss2[:], AF.Ln, [(ks, None)], [(ks, None)])
            yield
            self.act(ss2[:], ss2[:], AF.Exp, [(ks, None)], [(ks, None)], scale=-0.5)
            yield
            for j in range(2):
                h = hp * 2 + j
                self.stt("dve", ym[:, h * 128:(h + 1) * 128], hn[:, j, :], ss2[:, j:j + 1],
                         gate[:, c, h * 128:(h + 1) * 128], ALU.mult, ALU.mult,
                         [(kh, j), (ks, None), ("gate", c)], [(ymk[0], h)])
            yield

        for c in range(NCM):
            cs = slice(c * 128, (c + 1) * 128)
            ym, ymk = ymt[c % 2], ("ym%d" % (c % 2), None)
            gens = [hp_stream(c, 0, ym, ymk), hp_stream(c, 1, ym, ymk)]
            while gens:
                for g in list(gens):
                    try:
                        next(g)
                    except StopIteration:
                        gens.remove(g)
            pt, pk = self.npt()
            for h in range(4):
                self.tr(pt[:, h * 128:(h + 1) * 128], ym[:, h * 128:(h + 1) * 128], ident[:], [ymk, ("b_ident", None)], [pk])
            self.cp("act", yT[:, 0:4, cs], pt[:, 0:512].rearrange("p (h t) -> p h t", h=4), [pk], [("yT", ("m", c))])

    def rwkv(self):
        t = self.t
        w_in, ident, id4 = t["w_in"], t["b_ident"], t["id4"]
        mu, pp, omka = t["c_mu15"], t["c_pp"], t["omka"]
        S32, Sb, yT = t["S32"], t["Sb"], t["yT"]
        Bbd, Kbd, ARbd, Vbdt = t["Bbd"], t["Kbd"], t["ARbd"], t["Vbdt"]
        mXA, mL = t["c_mXA"], t["c_mL"]
        txw = self.ab("txw", [128, TT])
        xab = self.ab("xab", [128, TT])
        sxg = self.ab("sxg", [128, TT])
        kq = self.ab("kq", [128, TT])
        prod = self.ab("prod", [128, 4, TT])
        rnat = self.ab("rnat", [128, 4, TT])
        vnat = self.ab("vnat", [128, 4, TT])
        Vbd = self.ab("Vbd", [128, NCR, 4, 128])
        zt = [self.af("rz%d" % i, [128, TT + 8]) for i in range(2)]
        dt_ = [self.af("rd%d" % i, [128, TT]) for i in range(2)]
        PL = self.af("PL", [128, 4, NCR])
        names = ["R32", "K32", "V32", "sg", "cs", "Ep", "Em", "Epv", "KK", "tq", "KM"]
        F = {n: self.af("r_" + n, [128, TT]) for n in names}
        FK = {n: ("r_" + n, None) for n in names}
        zrr = [0]

        def lerp(items):
            grp = []
            for (j, dest, dk) in items:
                off, M = RW[j]
                ps, pk = self.nps()
                self.proj_fm("w_in", w_in, off, M, ps, pk, h0=H - 8)
                i = zrr[0] % 2
                zrr[0] += 1
                z, zk, d, dk_ = zt[i], ("rz%d" % i, None), dt_[i], ("rd%d" % i, None)
                self.act(z[0:M, 0:TT + 8], ps[0:M, 0:TT + 8], AF.Copy, [pk], [zk])
                grp.append((j, M, dest, dk, z, zk, d, dk_))
            for (j, M, dest, dk, z, zk, d, dk_) in grp:
                self.tt("dve", d[0:M, :], z[0:M, 7:TT + 7], z[0:M, 8:TT + 8], ALU.subtract, [zk], [dk_])
            for (j, M, dest, dk, z, zk, d, dk_) in grp:
                self.stt("dve", dest[0:M, :], d[0:M, :], mu[0:M, j:j + 1], z[0:M, 8:TT + 8], ALU.mult, ALU.add,
                         [dk_, zk, ("c_mu15", None)], [dk])

        lerp([(14, F["tq"], FK["tq"])])
        self.act(sxg[:], F["tq"][:], AF.Sigmoid, [FK["tq"]], [("sxg", None)])
        lerp([(12, F["tq"], FK["tq"]), (13, F["KM"], FK["KM"])])
        self.act(txw[0:64, :], F["tq"][0:64, :], AF.Tanh, [FK["tq"]], [("txw", None)])
        self.cp("act", xab[0:64, :], F["KM"][0:64, :], [FK["KM"]], [("xab", None)])
        sgs = [F["sg"]] + [self.af("r_sg%d" % i, [128, TT]) for i in range(1, 4)]
        sgk = [FK["sg"]] + [("r_sg%d" % i, None) for i in range(1, 4)]
        A32s = [self.ab("r_A%d" % i, [128, TT]) for i in range(4)]
        A32k = [("r_A%d" % i, None) for i in range(4)]
        for f in range(4):
            fs = slice(f * 128, (f + 1) * 128)
            psw, pkw = self.nps()
            self.mm(psw[:, 0:TT], t["w_up"][0:64, fs], txw[0:64, :], True, True, [("w_up", None), ("txw", None)], [pkw])
            psa, pka = self.nps()
            self.mm(psa[:, 0:TT], t["a_up"][0:64, fs], xab[0:64, :], True, True, [("a_up", None), ("xab", None)], [pka])
            self.act(sgs[f][:], psw[:, 0:TT], AF.Sigmoid, [pkw, ("c_pp", None)], [sgk[f]], bias=pp[:, 0, f:f + 1])
            self.act(A32s[f][:], psa[:, 0:TT], AF.Sigmoid, [pka, ("c_pp", None)], [A32k[f]], bias=pp[:, 1, f:f + 1])

        c3 = lambda ap: ap.rearrange("p (c t) -> p c t", t=LR)
        for f in range(4):
            R32, K32, V32, sg, cs, Ep, Em, Epv = (F[n] for n in ["R32", "K32", "V32", "sg", "cs", "Ep", "Em", "Epv"])
            KK, tq, KM = (F[n] for n in ["KK", "tq", "KM"])
            sg, A32 = sgs[f], A32s[f]
            FK = dict(FK)
            FK["sg"], FK["A32"] = sgk[f], A32k[f]
            lerp([(f, R32, FK["R32"]), (4 + f, K32, FK["K32"])])
            lerp([(8 + f, V32, FK["V32"])])
            fs = slice(f * 128, (f + 1) * 128)
            m64 = t["c_mask64"]
            self.act(kq[:], K32[:], AF.Square, [FK["K32"], ("c_pp", None)], [("kq", None)], scale=pp[:, 2, f:f + 1])
            pss_, pks_ = self.nps()
            self.mm(pss_[:, 0:TT], t["b_blockones"][:], kq[:], True, True, [("b_blockones", None), ("kq", None)], [pks_])
            self.ts("dve", tq[:], pss_[:, 0:TT], 1.0, 1e-24, ALU.mult, ALU.add, [pks_], [FK["tq"]])
            self.P.add("dve", lambda h, cs=cs, sg=sg, m64=m64: h.tensor_tensor_scan(cs[:], m64[:], sg[:], 0.0, ALU.mult, ALU.add),
                       [FK["sg"], ("c_mask64", None)], [FK["cs"]])
            self.act(tq[:], tq[:], AF.Ln, [FK["tq"]], [FK["tq"]])
            self.act(Ep[:], cs[:], AF.Exp, [FK["cs"]], [FK["Ep"]], scale=-DEC)
            self.act(tq[:], tq[:], AF.Exp, [FK["tq"]], [FK["tq"]], scale=-0.5)
            self.act(Em[:], cs[:], AF.Exp, [FK["cs"]], [FK["Em"]], scale=DEC)
            self.tt("dve", sg[:], cs[:], sg[:], ALU.subtract, [FK["cs"], FK["sg"]], [FK["sg"]])
            self.stt("dve", KK[:], K32[:], pp[:, 2, f:f + 1], tq[:], ALU.mult, ALU.mult,
                     [FK["K32"], FK["tq"], ("c_pp", None)], [FK["KK"]])
            self.act(Epv[:], sg[:], AF.Exp, [FK["sg"]], [FK["Epv"]], scale=-DEC)
            self.cp("act", PL[:, f, :], c3(Ep[:])[:, :, LR - 1], [FK["Ep"]], [("PL", f)])
            self.tt("dve", rnat[:, f, :], R32[:], Ep[:], ALU.mult, [FK["R32"], FK["Ep"]], [("rnat", f)])
            self.ts("dve", tq[:], A32[:], pp[:, 3, f:f + 1], omka[:, f:f + 1], ALU.mult, ALU.add,
                    [FK["A32"], ("c_pp", None), ("omka", None)], [FK["tq"]])
            self.cp("act", vnat[:, f, :], V32[:], [FK["V32"]], [("vnat", f)])
            self.tt("dve", KM[:], K32[:], tq[:], ALU.mult, [FK["K32"], FK["tq"]], [FK["KM"]])
            self.tt("dve", tq[:], KK[:], A32[:], ALU.mult, [FK["KK"], FK["A32"]], [FK["tq"]])
            self.stt("dve", prod[:, f, :], R32[:], pp[:, 4, f:f + 1], KM[:], ALU.mult, ALU.mult,
                     [FK["R32"], FK["KM"], ("c_pp", None)], [("prod", f)])
            for hh in range(2):
                rs = slice(hh * 64, hh * 64 + 64)
                cols = slice(hh * 64, hh * 64 + 64)
                cols_r = slice(128 + hh * 64, 128 + hh * 64 + 64)
                self.stt("dve", ARbd[rs, f, :, cols], c3(KK[rs, :]), -1.0, c3(Epv[rs, :]), ALU.mult, ALU.mult,
                         [FK["KK"], FK["Epv"]], [("ARbd", (f, "a", hh))])
                self.cp("act", ARbd[rs, f, :, cols_r], c3(rnat[rs, f, :]), [("rnat", f)], [("ARbd", (f, "r", hh))])
                self.tt("dve", Bbd[rs, f, :, cols], c3(tq[rs, :]), c3(Em[rs, :]), ALU.mult, [FK["tq"], FK["Em"]],
                        [("Bbd", (f, hh))])
                self.tt("dve", Kbd[rs, f, :, cols], c3(KM[rs, :]), c3(Em[rs, :]), ALU.mult, [FK["KM"], FK["Em"]],
                        [("Kbd", (f, hh))])
                self.cp("act", Vbdt[rs, f, :, cols], c3(vnat[rs, f, :]), [("vnat", f)], [("Vbdt", (f, hh))])
            pt, pk = self.npt()
            for c in range(NCR):
                self.tr(pt[:, c * 128:(c + 1) * 128], Vbdt[:, f, c, :], ident[:], [("Vbdt", None), ("b_ident", None)], [pk])
            self.cp("act", Vbd[:, :, f, :], pt[:, 0:NCR * 128].rearrange("p (c i) -> p c i", c=NCR), [pk], [("Vbd", f)])

        XA = self.ab("XA", [128, 4, 2, 128])
        KA = self.ab("KA", [128, 4, 2, 128])
        Xp = [self.ab("Xp%d" % i, [128, 4, 128]) for i in range(2)]
        Np = [self.ab("Np%d" % i, [128, 4, 128]) for i in range(2)]
        Rm = [self.ab("Rm%d" % i, [128, 4, 128]) for i in range(2)]
        tmpR = self.ab("tmpR", [128, 4, 128])
        XAs = self.ab("XAs", [128, 4, 64])
        KAs = self.ab("KAs", [128, 4, 64])
        BK = self.ab("BKtok", [128, 2, 4, 128])
        Wb = self.ab("Wb", [128, 4, 128])
        Ub = self.ab("Ub", [128, 4, 128])
        yr = self.ab("yr", [128, 512])
        Y32 = self.af("Y32", [128, 512])
        sq32 = self.af("sq32", [128, 512])
        st1 = self.af("st1", [128, 8])
        st2 = self.af("st2", [128, 8])
        st3 = self.af("st3", [128, 8])
        bon = self.af("bon", [128, 8])
        tS = self.af("tS", [128, 4, 128])
        hp4 = lambda ap: ap.rearrange("p (a b) -> p a b", a=4)
        lnb = t["c_ln_bc"]
        h8 = lambda ap: ap.rearrange("p (h i) -> p h i", h=8)
        Rfin = {}

        def prep(c):
            for pair in range(2):
                ps, pk = self.nps()
                for j in range(2):
                    hp = pair * 2 + j
                    self.mm(ps[:, j * 256:(j + 1) * 256], Bbd[:, hp, c, :], ARbd[:, hp, c, :], True, True,
                            [("Bbd", None), ("ARbd", None)], [pk])
                mbc = mXA[:].rearrange("p a b -> p (a b)").unsqueeze(1).to_broadcast([128, 2, 256])
                self.tt("dve", XA[:, pair * 2:pair * 2 + 2, :, :].rearrange("p j a b -> p j (a b)"),
                        ps[:, 0:512].rearrange("p (j n) -> p j n", j=2), mbc,
                        ALU.mult, [pk, ("c_mXA", None)], [("XA", pair * 2), ("XA", pair * 2 + 1)])
                ps, pk = self.nps()
                for j in range(2):
                    hp = pair * 2 + j
                    self.mm(ps[:, j * 256:(j + 1) * 256], Kbd[:, hp, c, :], ARbd[:, hp, c, :], True, True,
                            [("Kbd", None), ("ARbd", None)], [pk])
                self.tt("dve", KA[:, pair * 2:pair * 2 + 2, :, :].rearrange("p j a b -> p j (a b)"),
                        ps[:, 0:512].rearrange("p (j n) -> p j n", j=2), mbc,
                        ALU.mult, [pk, ("c_mXA", None)], [("KA", pair * 2), ("KA", pair * 2 + 1)])
            ps, pk = self.nps()
            for hp in range(4):
                self.mm(ps[:, hp * 128:(hp + 1) * 128], ARbd[:, hp, c, 0:128], Bbd[:, hp, c, :], True, True,
                        [("ARbd", None), ("Bbd", None)], [pk])
            self.tt("dve", Np[0][:], hp4(ps[:, :]), mL[:].unsqueeze(1).to_broadcast([128, 4, 128]), ALU.mult,
                    [pk, ("c_mL", None)], [("Np0", None)])
            self.tt("pool", XAs[:], XA[:, :, 1, 0:64], XA[:, :, 1, 64:128], ALU.add, [("XA", None)], [("XAs", None)])
            self.tt("pool", KAs[:], KA[:, :, 1, 0:64], KA[:, :, 1, 64:128], ALU.add, [("KA", None)], [("KAs", None)])

        def doubling(c):
            cur = 0
            for lvl in range(5):
                nx = 1 - cur
                if lvl == 0:
                    Xc, kXc, Rc, kRc = XA[:, :, 0, :], ("XA", None), XA[:, :, 0, :], ("XA", None)
                else:
                    Xc, kXc, Rc, kRc = Xp[cur], ("Xp%d" % cur, None), Rm[cur], ("Rm%d" % cur, None)
                Nc, kNc = Np[cur], ("Np%d" % cur, None)
                Xn, Nn, kXn, kNn = Xp[nx], Np[nx], ("Xp%d" % nx, None), ("Np%d" % nx, None)
                Rn, kRn = Rm[nx], ("Rm%d" % nx, None)
                ps, pk = self.nps()
                for hp in range(4):
                    self.mm(ps[:, hp * 128:(hp + 1) * 128], Nc[:, hp, :], Xc[:, hp, :], True, True, [kNc, kXc], [pk])
                self.cp("act", Xn[:], hp4(ps[:, :]), [pk], [kXn])
                ps, pk = self.nps()
                for hp in range(4):
                    self.mm(ps[:, hp * 128:(hp + 1) * 128], Xc[:, hp, :], Nc[:, hp, :], True, True, [kNc, kXc], [pk])
                self.cp("act", Nn[:], hp4(ps[:, :]), [pk], [kNn])
                self.tt("pool", tmpR[:], Rc, Xn[:], ALU.add, [kRc, kXn], [("tmpR", None)])
                ps, pk = self.nps()
                for hp in range(4):
                    self.mm(ps[:, hp * 128:(hp + 1) * 128], Nn[:, hp, :], Rc[:, hp, :], True, True, [kNn, kRc], [pk])
                self.tt("dve", Rn[:], hp4(ps[:, :]), tmpR[:], ALU.add, [pk, ("tmpR", None)], [kRn])
                cur = nx
                yield
            Rfin[c] = (Rm[cur], ("Rm%d" % cur, None))

        def bk(c):
            pt, pk = self.npt()
            for hp in range(4):
                self.tr(pt[:, hp * 128:(hp + 1) * 128], Bbd[:, hp, c, :], ident[:], [("Bbd", None), ("b_ident", None)], [pk])
            for hp in range(4):
                self.tr(pt[:, 512 + hp * 128:512 + (hp + 1) * 128], Kbd[:, hp, c, :], ident[:],
                        [("Kbd", None), ("b_ident", None)], [pk])
            self.cp("act", BK[:], pt[:, :].rearrange("p (a b c) -> p a b c", a=2, b=4), [pk], [("BKtok", None)])

        def chain(c):
            c64 = slice(c * LR, (c + 1) * LR)
            Rf, kRf = Rfin[c]
            ps, pk = self.nps()
            for hp in range(4):
                o = ps[:, hp * 128:(hp + 1) * 128]
                self.mm(o, ARbd[:, hp, c, 0:128], Sb[:, hp, :], True, False, [("ARbd", None), ("Sb", None)], [pk])
                self.mm(o, KA[:, hp, 0, :], Vbd[:, c, hp, :], False, True, [("KA", hp), ("Vbd", hp)], [pk])
            self.cp("act", Wb[:], hp4(ps[:, :]), [pk], [("Wb", None)])
            ps, pk = self.nps()
            for hp in range(4):
                self.mm(ps[:, hp * 128:(hp + 1) * 128], Rf[:, hp, :], Wb[:, hp, :], True, True, [kRf, ("Wb", None)], [pk])
            self.tt("dve", Ub[:], hp4(ps[:, :]), Wb[:], ALU.add, [pk, ("Wb", None)], [("Ub", None)])
            psy, pky = self.nps()
            for hp in range(4):
                o = psy[0:64, hp * 128:(hp + 1) * 128]
                self.mm(o, rnat[:, hp, c64], Sb[:, hp, :], True, False, [("rnat", hp), ("Sb", None)], [pky])
                self.mm(o, XAs[:, hp, :], Ub[:, hp, :], False, False, [("XAs", None), ("Ub", None)], [pky])
                self.mm(o, KAs[:, hp, :], Vbd[:, c, hp, :], False, True, [("KAs", None), ("Vbd", hp)], [pky])
            pss, pks = self.nps()
            for hp in range(4):
                o = pss[:, hp * 128:(hp + 1) * 128]
                self.mm(o, BK[:, 0, hp, :], Ub[:, hp, :], True, False, [("BKtok", None), ("Ub", None)], [pks])
                self.mm(o, BK[:, 1, hp, :], Vbd[:, c, hp, :], False, True, [("BKtok", None), ("Vbd", hp)], [pks])
            for hp in range(4):
                self.act(tS[:, hp, :], S32[:, hp, :], AF.Copy, [("S32", hp), ("PL", hp)], [("tS", hp)],
                         scale=PL[:, hp, c:c + 1])
                self.stt("dve", S32[:, hp, :], pss[:, hp * 128:(hp + 1) * 128], PL[:, hp, c:c + 1], tS[:, hp, :],
                         ALU.mult, ALU.add, [pks, ("PL", hp), ("tS", hp)], [("S32", hp)])
            self.cp("act", Sb[:], S32[:], [("S32", None)], [("Sb", None)])
            self.cp("act", Y32[0:64, :], psy[0:64, :], [pky], [("Y32", None)])

        def post(c):
            c64 = slice(c * LR, (c + 1) * LR)
            psb, pkb = self.nps()
            for f in range(4):
                self.mm(psb[0:64, 0:8], prod[:, f, c64], t["b_hsel"][:, f, :], f == 0, f == 3, [("prod", f), ("b_hsel", None)], [pkb])
            self.cp("act", bon[0:64, :], psb[0:64, 0:8], [pkb], [("bon", None)])
            ptv, pkv = self.npt()
            for f in range(4):
                self.tr(ptv[0:64, f * 128:(f + 1) * 128], vnat[:, f, c64], ident[:], [("vnat", f), ("b_ident", None)], [pkv])
            psg, pkg = self.psf[5], ("psf5", None)
            self.mm(psg[0:64, :], sxg[:, c64], t["g_up"][:, :], True, True, [("sxg", None), ("g_up", None)], [pkg])
            self.P.add("dve", lambda h: h.reduce_sum(st1[0:64, :], h8(Y32[0:64, :]), AX.X), [("Y32", None)], [("st1", None)])
            self.act(sq32[0:64, :], Y32[0:64, :], AF.Square, [("Y32", None)], [("sq32", None)])
            self.P.add("dve", lambda h: h.reduce_sum(st2[0:64, :], h8(sq32[0:64, :]), AX.X), [("sq32", None)], [("st2", None)])
            yield
            self.ts("dve", st1[0:64, :], st1[0:64, :], 1.0 / 64, None, ALU.mult, None, [("st1", None)], [("st1", None)])
            self.tt("dve", st3[0:64, :], st1[0:64, :], st1[0:64, :], ALU.mult, [("st1", None)], [("st3", None)])
            self.stt("dve", st2[0:64, :], st2[0:64, :], 1.0 / 64, st3[0:64, :], ALU.mult, ALU.subtract,
                     [("st2", None), ("st3", None)], [("st2", None)])
            self.rsqrt_small(st2[0:64, :], st2[0:64, :], 1.0, 64e-5, ("st2", None), ("st2", None))
            yield
            bc = lambda ap: ap.unsqueeze(2).to_broadcast([64, 8, 64])
            self.tt("dve", h8(Y32[0:64, :]), h8(Y32[0:64, :]), bc(st1[0:64, :]), ALU.subtract, [("Y32", None), ("st1", None)],
                    [("Y32", None)])
            yield
            self.tt("dve", h8(Y32[0:64, :]), h8(Y32[0:64, :]), bc(st2[0:64, :]), ALU.mult, [("Y32", None), ("st2", None)],
                    [("Y32", None)])
            self.tt("dve", Y32[0:64, :], Y32[0:64, :], lnb[0:64, 0, :], ALU.mult, [("Y32", None), ("c_ln_bc", None)], [("Y32", None)])
            self.tt("dve", Y32[0:64, :], Y32[0:64, :], lnb[0:64, 1, :], ALU.add, [("Y32", None), ("c_ln_bc", None)], [("Y32", None)])
            yield
            self.tt("dve", h8(sq32[0:64, :]), h8(ptv[0:64, 0:512]), bc(bon[0:64, :]), ALU.mult, [pkv, ("bon", None)], [("sq32", None)])
            self.tt("dve", Y32[0:64, :], Y32[0:64, :], sq32[0:64, :], ALU.add, [("Y32", None), ("sq32", None)], [("Y32", None)])
            self.tt("dve", yr[0:64, :], Y32[0:64, :], psg[0:64, :], ALU.mult, [("Y32", None), pkg], [("yr", None)])
            pt, pk = self.npt()
            for f in range(4):
                self.tr(pt[:, f * 64:(f + 1) * 64], yr[0:64, f * 128:(f + 1) * 128], ident[0:64, 0:64], [("yr", None), ("b_ident", None)], [pk])
            self.cp("act", yT[:, 4:8, c64], pt[:, 0:256].rearrange("p (f t) -> p f t", f=4), [pk], [("yT", ("r", c))])
            yield

        def drain(*gens):
            gens = [g for g in gens if g is not None]
            while gens:
                for g in list(gens):
                    try:
                        next(g)
                    except StopIteration:
                        gens.remove(g)

        prep(0)
        drain(doubling(0))
        bk(0)
        for c in range(NCR):
            chain(c)
            if c + 1 < NCR:
                prep(c + 1)
                drain(doubling(c + 1), post(c))
                bk(c + 1)
            else:
                drain(post(c))

    def phaseB(self):
        t = self.t
        nseq = self.nseq
        TB, NSB = 512, 4
        self.load_consts(["gains", "ident"])
        self.consts_bf(["ident"])
        onesq = self.ab("onesq", [128, 128])
        self.ms("dve", onesq[:], 1.0, [("onesq", None)])
        self.load_w("wq", "wq", D, D)
        self.load_w("wo", "wo", D, D)
        self.af("ht", [128, NSB, D])
        self.af("ssq", [128, NSB])
        self.af("rstd", [128, NSB])
        self.ab("junk", [128, D])
        self.ab("xs0", [128, D])
        self.ab("xs1", [128, D])
        uT = self.ab("uT", [128, KT, TB + H])
        KTx = self.ab("KTx", [128, nseq * 4, 2, MEMT])
        Vx = self.ab("Vx", [128, nseq * 2, D])
        Kmax2 = self.af("Kmax2", [128, nseq * 4])
        qT = self.ab("qT", [128, KT, TB])
        qsq = self.ab("qsq", [128, KT, TB])
        oT = self.ab("oT", [128, KT, TB])
        pTs = [[self.ab("pT%d_%d" % (u, i), [128, TB]) for i in range(2)] for u in range(2)]
        cbs = [self.af("cb%d" % u, [128, TB]) for u in range(2)]
        scs = [[self.af("sc%d_%d" % (u, i), [128, TB]) for i in range(2)] for u in range(2)]
        rinvs = [self.af("rinv%d" % u, [128, TB]) for u in range(2)]
        rinv = rinvs[0]
        mark = self.b_off
        wkv = self.load_w("wkv", "wkv", D, 2 * D)
        ksq = self.ab("ksq", [128, MEMT])
        ht = t["ht"]
        for b in range(nseq):
            mv = self.mem[b * MEMT:(b + 1) * MEMT, :].rearrange("(s p) d -> p s d", p=128)
            self.dma("sp", ht[:, 0:2, :], mv, [], [("ht", None)])
            self.norm_T(3)
            for h in range(4):
                psr, pkr = self.nps()
                for dt in range(2):
                    ps, pk = self.nps()
                    self.proj_fm("wkv", wkv, h * 256 + dt * 128, 128, ps, pk)
                    self.cp("act", KTx[:, b * 4 + h, dt, :], ps[:, 0:MEMT], [pk], [("KTx", (b, h, dt))])
                    self.act(ksq[:], ps[:, 0:MEMT], AF.Square, [pk], [("ksq", None)])
                    self.mm(psr[:, 0:MEMT], onesq[:], ksq[:], dt == 0, dt == 1, [("onesq", None), ("ksq", None)], [pkr])
                self.cp("act", rinv[:, 0:MEMT], psr[:, 0:MEMT], [pkr], [("rinv0", None)])
                self.P.add("dve", lambda h_, o=Kmax2[:, b * 4 + h:b * 4 + h + 1], i=rinv[:, 0:MEMT]: h_.reduce_max(o, i, AX.X),
                           [("rinv0", None)], [("Kmax2", (b, h))])
            for mt in range(2):
                for half in range(2):
                    ps, pk = self.nps()
                    self.proj_tm("wkv", wkv, KT, uT, "uT", mt, D + half * 512, 512, ps, pk, off=H)
                    self.cp("act", Vx[:, b * 2 + mt, half * 512:(half + 1) * 512], ps[:, :], [pk], [("Vx", (b, mt, half))])
        self.P.barrier()
        self.b_off = mark
        self.TT, self.NS = TB, NSB
        for b in range(nseq):
            for ti in range(self.T // TB):
                r0 = b * self.T + ti * TB
                hv = self.h1d[r0:r0 + TB, :].rearrange("(s p) d -> p s d", p=128)
                self.dma("sp", ht[:], hv, [("h1d", None)], [("ht", None)])
                self.norm_T(1)
                for f in range(KT):
                    ps, pk = self.nps()
                    self.proj_fm("wq", t["wq"], f * 128, 128, ps, pk)
                    self.cp("act", qT[:, f, :], ps[:, 0:TB], [pk], [("qT", f)])
                    self.act(qsq[:, f, :], ps[:, 0:TB], AF.Square, [pk], [("qsq", f)])
                def head_stream(h, u):
                    cb_, sc_, pT_, rinv_ = cbs[u], scs[u], pTs[u], rinvs[u]
                    kcb, krv = "cb%d" % u, "rinv%d" % u
                    psr, pkr = self.nps()
                    for dt in range(2):
                        self.mm(psr[:, 0:TB], onesq[:], qsq[:, 2 * h + dt, :], dt == 0, dt == 1,
                                [("onesq", None), ("qsq", 2 * h + dt)], [pkr])
                    self.ts("dve", cb_[:], psr[:, 0:TB], Kmax2[:, b * 4 + h:b * 4 + h + 1], -0.5, ALU.add, ALU.mult,
                            [pkr, ("Kmax2", None)], [(kcb, None)])
                    yield
                    for mt in range(2):
                        ms_ = slice(mt * 128, (mt + 1) * 128)
                        ps, pk = self.nps()
                        for dt in range(2):
                            self.mm(ps[:, 0:TB], KTx[:, b * 4 + h, dt, ms_], qT[:, 2 * h + dt, :], dt == 0, dt == 1,
                                    [("KTx", None), ("qT", 2 * h + dt)], [pk])
                        self.tt("dve", sc_[mt][:], ps[:, 0:TB], cb_[:], ALU.add, [pk, (kcb, None)], [("sc%d_%d" % (u, mt), None)])
                        self.act(pT_[mt][:], sc_[mt][:], AF.Exp, [("sc%d_%d" % (u, mt), None)], [("pT%d_%d" % (u, mt), None)],
                                 scale=1.0 / 16)
                        yield
                    psb, pkb = self.nps()
                    for mt in range(2):
                        self.mm(psb[:, 0:TB], onesq[:], pT_[mt][:], mt == 0, mt == 1,
                                [("onesq", None), ("pT%d_%d" % (u, mt), None)], [pkb])
                    self.cp("act", rinv_[:], psb[:, 0:TB], [pkb], [(krv, None)])
                    yield
                    self.P.add("dve", lambda h_, o=rinv_: h_.reciprocal(o[:], o[:]), [(krv, None)], [(krv, None)])
                    yield
                    for dt in range(2):
                        ps, pk = self.nps()
                        for mt in range(2):
                            self.mm(ps[:, 0:TB], Vx[:, b * 2 + mt, h * 256 + dt * 128:h * 256 + (dt + 1) * 128], pT_[mt][:],
                                    mt == 0, mt == 1, [("Vx", None), ("pT%d_%d" % (u, mt), None)], [pk])
                        self.tt("dve", oT[:, 2 * h + dt, :], ps[:, 0:TB], rinv_[:], ALU.mult, [pk, (krv, None)],
                                [("oT", 2 * h + dt)])
                        yield

                for hpair in range(2):
                    gens = [head_stream(hpair * 2, 0), head_stream(hpair * 2 + 1, 1)]
                    while gens:
                        for g in list(gens):
                            try:
                                next(g)
                            except StopIteration:
                                gens.remove(g)
                for s in range(NSB):
                    for half in range(2):
                        ps, pk = self.nps()
                        self.proj_tm("wo", t["wo"], KT, oT, "oT", s, half * 512, 512, ps, pk)
                        hv2 = ht[:, s, half * 512:(half + 1) * 512]
                        self.tt("dve", hv2, hv2, ps[:, :], ALU.add, [pk, ("ht", s)], [("ht", s)])
                hd = self.h2d[r0:r0 + TB, :].rearrange("(s p) d -> p s d", p=128)
                self.dma("sp", hd, ht[:], [("ht", None)], [("h2d", (b, ti))])
                if self.dbg and "h2" in self.dbg:
                    self.dma("act", self.dbgd["h2"][r0:r0 + TB, :].rearrange("(s p) d -> p s d", p=128), ht[:],
                             [("ht", None)], [("dbg_h2", (b, ti))])

        self.TT, self.NS = TT, NS

    def phaseC(self):
        t = self.t
        TC, NSC = 512, 4
        self.TT, self.NS = TC, NSC
        self.load_consts(["gains", "ident", "nf_bc"])
        self.consts_bf(["ident"])
        self.load_w("wg", "wg", D, DFF)
        self.load_w("wu", "wu", D, DFF)
        self.load_w("wd", "wd", DFF, D)
        ht = self.af("ht", [128, NSC, D])
        self.af("ssq", [128, NSC])
        self.af("rstd", [128, NSC])
        self.ab("junk", [128, D])
        self.ab("xs0", [128, D])
        self.ab("xs1", [128, D])
        self.ab("uT", [128, KT, TC + H])
        hT = self.ab("hT", [128, NFF, TC])
        sgt = [self.af("sgt%d" % i, [128, TC]) for i in range(2)]
        nf = t["c_nf_bc"]
        src = self.h2d if "B" in self.phases else (self.h1d if "A" in self.phases else self.x)
        ntok = self.nseq * self.T
        for r0 in range(0, ntok, TC):
            hv = src[r0:r0 + TC, :].rearrange("(s p) d -> p s d", p=128)
            self.dma("sp", ht[:], hv, [("h2d", None), ("h1d", None)], [("ht", None)])
            self.norm_T(2)
            for f in range(NFF):
                psg, pkg = self.nps()
                self.proj_fm("wg", t["wg"], f * 128, 128, psg, pkg)
                psu, pku = self.nps()
                self.proj_fm("wu", t["wu"], f * 128, 128, psu, pku)
                sg, sk = sgt[f % 2], ("sgt%d" % (f % 2), None)
                self.act(sg[:], psg[:, 0:TC], AF.Silu, [pkg], [sk])
                self.tt("dve", hT[:, f, :], sg[:], psu[:, 0:TC], ALU.mult, [sk, pku], [("hT", f)])
            for s in range(NSC):
                for half in range(2):
                    ps, pk = self.nps()
                    self.proj_tm("wd", t["wd"], NFF, hT, "hT", s, half * 512, 512, ps, pk)
                    hv2 = ht[:, s, half * 512:(half + 1) * 512]
                    self.tt("dve", hv2, hv2, ps[:, :], ALU.add, [pk, ("ht", s)], [("ht", s)])
            for s in range(NSC):
                self.act(t["junk"][:], ht[:, s, :], AF.Square, [("ht", s)], [("junk", None), ("ssq", s)],
                         accum=t["ssq"][:, s:s + 1])
            self.rsqrt_small(t["rstd"][:], t["ssq"][:], 1.0 / D, EPS, ("rstd", None), ("ssq", None))
            for s in range(NSC):
                self.stt("dve", ht[:, s, :], ht[:, s, :], t["rstd"][:, s:s + 1], nf[:], ALU.mult, ALU.mult,
                         [("ht", s), ("rstd", None), ("c_nf_bc", None)], [("ht", s)])
            ov = self.out[r0:r0 + TC, :].rearrange("(s p) d -> p s d", p=128)
            self.dma("sp", ov, ht[:], [("ht", None)], [("out_d", r0)])
        self.TT, self.NS = TT, NS


def host_params(inp):
    f32 = np.float32
    g = np.stack([inp["norm_mix"][0], inp["norm_xattn"][0], inp["norm_ffn"][0], inp["norm_mem"][0]])
    d = {}
    d["gains"] = np.ascontiguousarray(g.reshape(4, KT, 128).transpose(2, 0, 1))
    d["convw"] = np.ascontiguousarray(inp["mlstm_conv"][0].T.reshape(8, 128, 4).transpose(1, 0, 2))
    d["gb"] = np.ascontiguousarray(np.stack([inp["mlstm_i_bias"][0], inp["mlstm_f_bias"][0]], axis=1))
    d["nw_bc"] = np.ascontiguousarray(np.broadcast_to(inp["mlstm_norm"][0][None, :], (128, 512)))
    mu = inp["rwkv_mu"][0]
    mu15 = np.zeros((128, 15), f32)
    for j, (off, M) in enumerate(RW):
        mu15[:M, j] = mu[off - OFF_R:off - OFF_R + M]
    d["mu15"] = mu15
    pp = np.zeros((128, 6, 4), f32)
    for i, k in enumerate(["rwkv_w0", "rwkv_a0", "rwkv_k_k", "rwkv_k_a", "rwkv_r_k"]):
        pp[:, i, :] = inp[k][0].reshape(4, 128).T
    d["pp"] = pp
    d["ln_bc"] = np.ascontiguousarray(np.broadcast_to(
        np.stack([inp["rwkv_ln_w"][0], inp["rwkv_ln_b"][0]])[None], (128, 2, 512)))
    d["nf_bc"] = np.ascontiguousarray(np.broadcast_to(inp["norm_final"][None, :], (128, D)))
    i128 = np.arange(128)
    d["ident"] = np.eye(128, dtype=f32)
    d["maskU"] = (i128[:, None] <= i128[None, :]).astype(f32)
    l64 = i128 % 64
    strict = (l64[:, None] < l64[None, :]).astype(f32)
    incl = (l64[:, None] <= l64[None, :]).astype(f32)
    d["mXA"] = np.ascontiguousarray(np.stack([strict, incl], axis=1))
    d["mL"] = (l64[:, None] > l64[None, :]).astype(f32)
    sel = np.zeros((4, 8, 128), f32)
    for h in range(4):
        sel[h, h, :] = 1.0
        sel[h, 4 + h, :] = 128.0 ** -0.5
    d["sel"] = sel
    tt_ = np.arange(TT)
    d["mask4"] = np.ascontiguousarray(np.broadcast_to((tt_ % LM != 0).astype(f32)[None], (4, TT)))
    d["mask64"] = np.ascontiguousarray(np.broadcast_to((tt_ % LR != 0).astype(f32)[None], (128, TT)))
    d["blockones"] = ((i128[:, None] // 64) == (i128[None, :] // 64)).astype(f32)
    hsel = np.zeros((128, 4, 8), f32)
    for f in range(4):
        hsel[:64, f, 2 * f] = 1.0
        hsel[64:, f, 2 * f + 1] = 1.0
    d["hsel"] = hsel
    w = {"w_in": "w_in", "w_mix": "w_mix_out", "wq": "xattn_wq", "wkv": "xattn_wkv", "wo": "xattn_wo",
         "wg": "ffn_w_gate", "wu": "ffn_w_up", "wd": "ffn_w_down", "w_up": "rwkv_w_up", "a_up": "rwkv_a_up",
         "g_up": "rwkv_g_up"}
    for k, src in w.items():
        d[k] = np.ascontiguousarray(inp[src][0])
    for k in d:
        d[k] = np.ascontiguousarray(d[k], dtype=f32)
    return d


def kernel(**inp):
    ncores = 8
    B, T, _ = inp["x"].shape
    nseq = B // ncores
    kb = K(nseq, T)
    nc = kb.build()
    shared = host_params(inp)
    in_maps = []
    for c in range(ncores):
        m = dict(shared)
        m["x"] = np.ascontiguousarray(inp["x"][c * nseq:(c + 1) * nseq].reshape(nseq * T, D), dtype=np.float32)
        m["mem"] = np.ascontiguousarray(inp["mem"][c * nseq:(c + 1) * nseq].reshape(nseq * MEMT, D), dtype=np.float32)
        in_maps.append(m)
    res = run_bass_kernel_spmd(nc, in_maps, core_ids=list(range(ncores)))
    out = np.concatenate([np.asarray(r["out"]).reshape(nseq, T, D) for r in res.results], axis=0)
    return out.astype(np.float32)
```
